# Optimizing a Trainium2 kernel written in Bass

```python
import math
import jax, jax.numpy as jnp
from jax import lax
import numpy as np

D_MODEL = 2048
BATCH = 4
SEQ = 2048
DEPTH = 4

MIX_WIDTH = D_MODEL
SSM_WIDTH = D_MODEL // 2
CONV_WIDTH = MIX_WIDTH - SSM_WIDTH
SSM_CH_PER_GROUP = 16
SSM_GROUPS = SSM_WIDTH // SSM_CH_PER_GROUP
SSM_STATE = 64
IN_WIDTH = SSM_WIDTH + 2 * CONV_WIDTH
CONV_K = 31
N_EXPERT_GROUPS = 4
EXPERTS_PER_GROUP = 8
N_EXPERTS = N_EXPERT_GROUPS * EXPERTS_PER_GROUP
EXPERT_TOP_K = 2
EXPERT_HIDDEN = D_MODEL // 8
PLE_DIM = 256
DEEPNORM_ALPHA = (2 * DEPTH) ** 0.25
DEEPNORM_BETA = (8 * DEPTH) ** -0.25
LN_EPS = 1e-5
LAMBDA_RE_MAX = -1e-4
DT_MIN = 1e-3
DT_MAX = 1e-1

kernel_name = 'hymba_s5_conformer_hmoe_deepnorm'


def layer_norm(x, g, b):
    xf = x.astype(jnp.float32)
    mu = jnp.mean(xf, axis=-1, keepdims=True)
    xc = xf - mu
    var = jnp.mean(xc * xc, axis=-1, keepdims=True)
    y = xc * lax.rsqrt(var + LN_EPS) * g.astype(jnp.float32) + b.astype(jnp.float32)
    return y.astype(x.dtype)


def s5_group(u, lam_re, lam_im, log_dt, b_re, b_im, c_re, c_im, d_skip, w_glu, b_glu):
    bsz, seqlen, _ = u.shape
    f32 = jnp.float32
    uf = u.astype(f32).reshape(bsz, seqlen, SSM_GROUPS, SSM_CH_PER_GROUP)
    lam = lax.complex(jnp.minimum(lam_re.astype(f32), LAMBDA_RE_MAX), lam_im.astype(f32))
    dt = jnp.exp(log_dt.astype(f32))[:, None]
    lam_bar = jnp.exp(lam * dt)
    b_c = lax.complex(b_re.astype(f32), b_im.astype(f32))
    b_bar = ((lam_bar - 1.0) / lam)[..., None] * b_c
    bu = jnp.einsum('blgh,gph->blgp', uf.astype(jnp.complex64), b_bar)
    a = jnp.broadcast_to(lam_bar[None, None], (1, seqlen, SSM_GROUPS, SSM_STATE))

    def combine(left, right):
        a1, s1 = left
        a2, s2 = right
        return a1 * a2, a2 * s1 + s2

    _, states = lax.associative_scan(combine, (a, bu), axis=1)
    c_c = lax.complex(c_re.astype(f32), c_im.astype(f32))
    y = jnp.einsum('blgp,ghp->blgh', states, c_c).real
    y = y + d_skip.astype(f32).reshape(SSM_GROUPS, SSM_CH_PER_GROUP) * uf
    y = y.reshape(bsz, seqlen, SSM_WIDTH)
    z = jax.nn.gelu(y)
    out = z * jax.nn.sigmoid(z @ w_glu.astype(f32) + b_glu.astype(f32))
    return out.astype(u.dtype)


def conformer_conv_group(v, g, w_dw, b_dw, ln_g, ln_b):
    h = v * jax.nn.sigmoid(g)
    h = lax.conv_general_dilated(h, w_dw[:, None, :], window_strides=(1,),
                                 padding=[(CONV_K - 1, 0)],
                                 dimension_numbers=('NWC', 'WIO', 'NWC'),
                                 feature_group_count=CONV_WIDTH) + b_dw
    h = layer_norm(h, ln_g, ln_b)
    return jax.nn.silu(h)


def hierarchical_moe(h, w_rg, b_rg, w_re, b_re, w_gate, w_up, w_down):
    bsz, seqlen, dm = h.shape
    n_tok = bsz * seqlen
    t = h.reshape(n_tok, dm)
    g_prob = jax.nn.softmax((t @ w_rg + b_rg).astype(jnp.float32), axis=-1)
    g_p, g_idx = lax.top_k(g_prob, 1)
    e_logits = (t @ w_re + b_re).astype(jnp.float32).reshape(n_tok, N_EXPERT_GROUPS, EXPERTS_PER_GROUP)
    sel = jnp.broadcast_to(g_idx[:, :, None], (n_tok, 1, EXPERTS_PER_GROUP))
    e_sel = jnp.take_along_axis(e_logits, sel, axis=1)[:, 0]
    e_val, e_idx = lax.top_k(e_sel, EXPERT_TOP_K)
    e_w = jax.nn.softmax(e_val, axis=-1) * g_p
    expert_id = g_idx * EXPERTS_PER_GROUP + e_idx
    combine_w = jnp.sum(jax.nn.one_hot(expert_id, N_EXPERTS, dtype=jnp.float32) * e_w[..., None], axis=1)
    gate = jnp.einsum('nd,edf->nef', t, w_gate)
    up = jnp.einsum('nd,edf->nef', t, w_up)
    act = jax.nn.silu(gate) * up * combine_w[..., None].astype(t.dtype)
    out = jnp.einsum('nef,efd->nd', act, w_down)
    return out.reshape(bsz, seqlen, dm)


def setup_inputs(seed: int = 0) -> dict:
    key = jax.random.key(seed)
    ks = jax.random.split(key, 36)
    f32 = jnp.float32

    def nrm(k, shape, scale):
        return scale * jax.random.normal(k, shape, f32)

    L, G, P, CH = DEPTH, SSM_GROUPS, SSM_STATE, SSM_CH_PER_GROUP
    return {
        'x': nrm(ks[0], (BATCH, SEQ, D_MODEL), 1.0),
        'p': nrm(ks[1], (DEPTH, BATCH, SEQ, PLE_DIM), 1.0),
        'w_in': nrm(ks[2], (L, D_MODEL, IN_WIDTH), D_MODEL ** -0.5),
        'b_in': nrm(ks[3], (L, IN_WIDTH), 0.01),
        'lam_re': -0.5 + nrm(ks[4], (L, G, P), 0.01),
        'lam_im': jnp.pi * jnp.arange(P, dtype=f32)[None, None, :] + nrm(ks[5], (L, G, P), 0.01),
        'log_dt': jax.random.uniform(ks[6], (L, G), f32, math.log(DT_MIN), math.log(DT_MAX)),
        'ssm_b_re': nrm(ks[7], (L, G, P, CH), (2 * CH) ** -0.5),
        'ssm_b_im': nrm(ks[8], (L, G, P, CH), (2 * CH) ** -0.5),
        'ssm_c_re': nrm(ks[9], (L, G, CH, P), (2 * P) ** -0.5),
        'ssm_c_im': nrm(ks[10], (L, G, CH, P), (2 * P) ** -0.5),
        'ssm_d': nrm(ks[11], (L, SSM_WIDTH), 1.0),
        'w_glu': nrm(ks[12], (L, SSM_WIDTH, SSM_WIDTH), SSM_WIDTH ** -0.5),
        'b_glu': nrm(ks[13], (L, SSM_WIDTH), 0.01),
        'w_dw': nrm(ks[14], (L, CONV_K, CONV_WIDTH), CONV_K ** -0.5),
        'b_dw': nrm(ks[15], (L, CONV_WIDTH), 0.01),
        'conv_ln_g': 1.0 + nrm(ks[16], (L, CONV_WIDTH), 0.01),
        'conv_ln_b': nrm(ks[17], (L, CONV_WIDTH), 0.01),
        'w_o': nrm(ks[18], (L, MIX_WIDTH, D_MODEL), DEEPNORM_BETA * MIX_WIDTH ** -0.5),
        'b_o': nrm(ks[19], (L, D_MODEL), 0.01),
        'ln1_g': 1.0 + nrm(ks[20], (L, D_MODEL), 0.01),
        'ln1_b': nrm(ks[21], (L, D_MODEL), 0.01),
        'w_rg': nrm(ks[22], (L, D_MODEL, N_EXPERT_GROUPS), D_MODEL ** -0.5),
        'b_rg': nrm(ks[23], (L, N_EXPERT_GROUPS), 0.01),
        'w_re': nrm(ks[24], (L, D_MODEL, N_EXPERTS), D_MODEL ** -0.5),
        'b_re': nrm(ks[25], (L, N_EXPERTS), 0.01),
        'w_gate': nrm(ks[26], (L, N_EXPERTS, D_MODEL, EXPERT_HIDDEN), D_MODEL ** -0.5),
        'w_up': nrm(ks[27], (L, N_EXPERTS, D_MODEL, EXPERT_HIDDEN), D_MODEL ** -0.5),
        'w_down': nrm(ks[28], (L, N_EXPERTS, EXPERT_HIDDEN, D_MODEL), DEEPNORM_BETA * EXPERT_HIDDEN ** -0.5),
        'ln2_g': 1.0 + nrm(ks[29], (L, D_MODEL), 0.01),
        'ln2_b': nrm(ks[30], (L, D_MODEL), 0.01),
        'w_p': nrm(ks[31], (L, PLE_DIM, D_MODEL), DEEPNORM_BETA * PLE_DIM ** -0.5),
        'w_pg': nrm(ks[32], (L, D_MODEL, D_MODEL), D_MODEL ** -0.5),
        'b_pg': nrm(ks[33], (L, D_MODEL), 0.01),
        'ln3_g': 1.0 + nrm(ks[34], (L, D_MODEL), 0.01),
        'ln3_b': nrm(ks[35], (L, D_MODEL), 0.01),
    }


def reference(x, p, w_in, b_in, lam_re, lam_im, log_dt, ssm_b_re, ssm_b_im, ssm_c_re, ssm_c_im,
              ssm_d, w_glu, b_glu, w_dw, b_dw, conv_ln_g, conv_ln_b, w_o, b_o, ln1_g, ln1_b,
              w_rg, b_rg, w_re, b_re, w_gate, w_up, w_down, ln2_g, ln2_b, w_p, w_pg, b_pg,
              ln3_g, ln3_b):
    for i in range(DEPTH):
        z = x @ w_in[i] + b_in[i]
        u_ssm = z[..., :SSM_WIDTH]
        v_conv = z[..., SSM_WIDTH:SSM_WIDTH + CONV_WIDTH]
        g_conv = z[..., SSM_WIDTH + CONV_WIDTH:]
        y_ssm = s5_group(u_ssm, lam_re[i], lam_im[i], log_dt[i], ssm_b_re[i], ssm_b_im[i],
                         ssm_c_re[i], ssm_c_im[i], ssm_d[i], w_glu[i], b_glu[i])
        y_conv = conformer_conv_group(v_conv, g_conv, w_dw[i], b_dw[i], conv_ln_g[i], conv_ln_b[i])
        y = jnp.concatenate([y_ssm, y_conv], axis=-1) @ w_o[i] + b_o[i]
        h = layer_norm(DEEPNORM_ALPHA * x + y, ln1_g[i], ln1_b[i])
        m = hierarchical_moe(h, w_rg[i], b_rg[i], w_re[i], b_re[i], w_gate[i], w_up[i], w_down[i])
        h = layer_norm(DEEPNORM_ALPHA * h + m, ln2_g[i], ln2_b[i])
        e = (p[i] @ w_p[i]) * jax.nn.sigmoid(h @ w_pg[i] + b_pg[i])
        x = layer_norm(DEEPNORM_ALPHA * h + e, ln3_g[i], ln3_b[i])
    return x
```

```python
from contextlib import ExitStack
import math
import numpy as np
import concourse.bass as bass
import concourse.mybir as mybir
from concourse.bass_utils import run_bass_kernel_spmd

F32 = mybir.dt.float32
BF16 = mybir.dt.bfloat16
ALU = mybir.AluOpType
AF = mybir.ActivationFunctionType
AX = mybir.AxisListType

NL = 4
DM = 2048
TH = 1024
NCORE = 4
ALPHA = float((2 * NL) ** 0.25)
EPS = 1e-5
MAGIC = 12582912.0
TWO_PI = float(2 * math.pi)
C_ID, C_SGN, C_MASK, C_IOTA, C_HPI, C_ONE, C_ONES, C_MAG, C_NMAG = 0, 128, 129, 137, 1161, 1162, 1163, 1291, 1292
NCON = 1293
V_BIN, V_D, V_BGLU, V_BDW, V_CLG, V_CLB, V_BO, V_L1G, V_L1B, V_L2G, V_L2B, V_BPG, V_L3G, V_L3B = \
    0, 24, 32, 40, 48, 56, 64, 80, 96, 112, 128, 144, 160, 176
NV = 192
ENGS = ("pe", "act", "dve", "pool", "sp")


class TR:
    def __init__(self):
        self.q = {e: [] for e in ENGS}
        self.cnt = {}
        self.lastw = {}
        self.readers = {}
        self.seen = {e: {} for e in ENGS}
        self.deferred = {e: [] for e in ENGS}
        self.fence = {}
        self.layer = 0

    def engsem(self, eng):
        return "s_%s_%d" % (eng, self.layer)

    def _waits(self, eng, R, W):
        waits = {}

        def need(tok):
            s, v, e = tok
            if e == eng and eng == "pe":
                return
            if self.seen[eng].get(s, 0) >= v:
                return
            if waits.get(s, 0) < v:
                waits[s] = v

        def needd(d):
            for s, (v, e) in d.items():
                need((s, v, e))

        for k in list(R) + list(W):
            f = self.fence.get(k[0])
            if f:
                needd(f)
        for k in R:
            t = self.lastw.get(k)
            if t:
                need(t)
        for k in W:
            t = self.lastw.get(k)
            if t:
                need(t)
            needd(self.readers.get(k, {}))
        for s, v in waits.items():
            self.seen[eng][s] = v
        return list(waits.items())

    def _reg(self, tok, R, W):
        s, v, e = tok
        for k in R:
            self.readers.setdefault(k, {})[s] = (v, e)
        for k in W:
            self.lastw[k] = tok
            self.readers[k] = {}

    def op(self, eng, fn, R=(), W=(), sig=True):
        waits = self._waits(eng, R, W)
        tok = None
        if sig:
            s = self.engsem(eng)
            self.cnt[s] = self.cnt.get(s, 0) + 1
            tok = (s, self.cnt[s], eng)
            for (r_, w_) in self.deferred[eng]:
                self._reg(tok, r_, w_)
            self.deferred[eng] = []
            self._reg(tok, R, W)
        else:
            self.deferred[eng].append((tuple(R), tuple(W)))
        self.q[eng].append((waits, fn, tok, 1))

    def dma(self, eng, out, in_, sem, R=(), W=()):
        waits = self._waits(eng, R, W)
        self.cnt[sem] = self.cnt.get(sem, 0) + 16
        tok = (sem, self.cnt[sem], "dma")
        self._reg(tok, R, W)
        self.q[eng].append((waits, lambda e: e.dma_start(out=out, in_=in_), tok, 16))

    def retire(self, arena):
        f = self.fence.setdefault(arena, {})

        def add(s, v, e):
            if f.get(s, (0, None))[0] < v:
                f[s] = (v, e)

        for k in [k for k in self.lastw if k[0] == arena]:
            s, v, e = self.lastw.pop(k)
            add(s, v, e)
        for k in [k for k in self.readers if k[0] == arena]:
            for s, (v, e) in self.readers.pop(k).items():
                add(s, v, e)

    def final_wait(self, eng):
        waits = []
        for s, v in self.cnt.items():
            if self.seen[eng].get(s, 0) < v:
                waits.append((s, v))
        self.q[eng].append((waits, None, None, 0))


def build_program():
    nc = bass.Bass("TRN2", target_bir_lowering=False)
    tr = TR()
    es = ExitStack()

    def din(name, shape, dt=F32):
        return nc.dram_tensor(name, shape, dt, kind="ExternalInput").ap()

    xT = din("xT", [2, DM, TH])
    pT = din("pT", [NL, 2, 256, TH])
    w_in = din("w_in", [NL, DM, 3072])
    w_glu = din("w_glu", [NL, 1024, 1024])
    w_o = din("w_o", [NL, DM, DM])
    w_gate = din("w_gate", [NL, 32, DM, 256])
    w_up = din("w_up", [NL, 32, DM, 256])
    w_down = din("w_down", [NL, 32, 256, DM])
    w_p = din("w_p", [NL, 256, DM])
    w_pg = din("w_pg", [NL, DM, DM])
    wr = din("wr", [NL, DM, 36])
    br = din("br", [NL, 128, 36])
    vecs = din("vecs", [NL, 128, NV])
    wdw = din("wdw", [NL, 128, 8 * 31])
    lamre = din("lamre", [NL, 128, 64])
    lamim = din("lamim", [NL, 128, 64])
    logdt = din("logdt", [NL, 128, 64])
    bstk = din("bstk", [NL, 128, 1024])
    bswp = din("bswp", [NL, 128, 1024])
    cn1 = din("cn1", [NL, 128, 1024])
    cn2 = din("cn2", [NL, 128, 1024])
    consts = din("consts", [128, NCON])
    outT = nc.dram_tensor("outT", [2, DM, TH], F32, kind="ExternalOutput").ap()
    xs = nc.dram_tensor("xs", [2, DM, TH], F32, kind="Internal").ap()

    def sb(name, shape, dt):
        return es.enter_context(nc.sbuf_tensor(name, shape, dt))

    xres = sb("xres", [128, 16, TH], F32)
    a1 = sb("a1", [128, 8, TH], F32)
    a2 = sb("a2", [128, 4, TH], F32)
    a3 = sb("a3", [128, 8, 1056], BF16)
    wa = sb("wa", [128, 3, 16 * 256], BF16)
    wb = sb("wb", [128, 2, 8 * 128], BF16)
    tq = sb("tq", [128, 3, 512], F32)
    bl = sb("bl", [128, 4, 8 * 128], BF16)
    cl = sb("cl", [128, 4, 8 * 128], BF16)
    con = sb("con", [128, NCON], F32)
    idb = sb("idb", [128, 128], BF16)
    oneb = sb("oneb", [128, 128], BF16)
    vec = sb("vec", [128, NV], F32)
    wdwt = sb("wdwt", [128, 8, 31], F32)
    ssmp = sb("ssmp", [128, 16, 64], F32)
    b1 = sb("b1", [128, 1024], BF16)
    cnt1 = sb("cnt1", [128, 1024], BF16)
    cnt2 = sb("cnt2", [128, 1024], BF16)
    carry = sb("carry", [128, 64], F32)
    halo = sb("halo", [128, 8, 30], BF16)
    wrt = sb("wrt", [128, 16, 36], BF16)
    brt = sb("brt", [128, 36], F32)
    rt = sb("rt", [128, 8, 36], F32)
    dgt = sb("dgt", [128, 31, 128], BF16)
    ps = es.enter_context(nc.psum_tensor("ps", [128, 8, 512], F32))

    a1f = a1
    xbv = a1[:, :, :].rearrange("p t n -> p (t n)").bitcast(BF16).rearrange("p (t n) -> p t n", t=16)
    uv = a2[:, :, :].rearrange("p t n -> p (t n)").bitcast(BF16).rearrange("p (t n) -> p t n", t=8)
    osv = a1[:, 4:8, :].rearrange("p t n -> p (t n)").bitcast(BF16).rearrange("p (t n) -> p t n", t=8)
    a3w = a3[:, :, :].rearrange("p t n -> p (t n)").bitcast(F32)
    cwT, cwm = a3w[0:32, 0:1024], a3w[0:32, 1024:2048]
    ptb = a2[:, 3, :].bitcast(BF16).rearrange("p (k n) -> p k n", k=2)
    wpd = a2[:, 0:2, :].rearrange("p t n -> p (t n)").bitcast(BF16).rearrange("p (k n) -> p k n", k=2)

    def actv(e8, ft):
        return uv[:, e8 * 2 + ft, :]

    def cc(c0, n=1):
        return con[:, c0:c0 + n]

    def vc(c0, n=1):
        return vec[:, c0:c0 + n]

    dve, act, pe, pool, sp = "dve", "act", "pe", "pool", "sp"
    OP = tr.op

    tr.dma(sp, con[:], consts[:], "d_con", W=[("con",)])
    OP(dve, lambda e: e.tensor_copy(out=idb[:], in_=con[:, C_ID:C_ID + 128]), R=[("con",)], W=[("idb",)])
    OP(dve, lambda e: e.memset(oneb[:], 1.0), W=[("oneb",)])
    OP(dve, lambda e: e.memset(cl[:], 0.0), W=[("cl",)])
    OP(dve, lambda e: e.memset(a3[:], 0.0), W=[("a3", "all")])
    CON = [("con",)]

    wa_i = [0]

    def wa_slot():
        i = wa_i[0] % 3
        wa_i[0] += 1
        return i

    wb_i = [0]

    def gemm(l, h, wsrc_fn, nkt, nmt, rhs_fn, rkeys_fn, evac_fn, mt_per_dma, banks=(0, 1)):
        cnt = 0
        for mb in range(0, nmt, mt_per_dma):
            s = wa_slot()
            ncols = mt_per_dma * 128
            dst = wa[:, s, 0:nkt * ncols].rearrange("p (k n) -> p k n", k=nkt)
            tr.dma(pool, dst, wsrc_fn(mb * 128, ncols), "d_wa%d" % s, W=[("wa", s)])
            for mi in range(mt_per_dma):
                mt = mb + mi
                for tb in range(2):
                    bank = banks[cnt % 2]
                    cnt += 1
                    for kt in range(nkt):
                        lhsT = dst[:, kt, mi * 128:(mi + 1) * 128]
                        rhs = rhs_fn(kt, tb)
                        last = kt == nkt - 1
                        OP(pe, (lambda e, o=ps[:, bank, :], a=lhsT, b=rhs, st=(kt == 0), sp_=last:
                                e.matmul(o, a, b, start=st, stop=sp_)),
                           R=[("wa", s)] + rkeys_fn(kt, tb), W=[("ps", bank)], sig=last)
                    evac_fn(mt, tb, bank)

    def tbs(tb):
        return slice(tb * 512, (tb + 1) * 512)

    for l in range(NL):
        tr.layer = l
        tr.dma(sp, vec[:], vecs[l], "d_vec", W=[("vec",)])
        tr.dma(sp, wdwt[:].rearrange("p t k -> p (t k)"), wdw[l], "d_wdw", W=[("wdw",)])
        tr.dma(sp, ssmp[:, 0, :], lamre[l], "d_ssm", W=[("ssmp", 0)])
        tr.dma(sp, ssmp[:, 1, :], lamim[l], "d_ssm", W=[("ssmp", 1)])
        tr.dma(sp, ssmp[:, 2, :], logdt[l], "d_ssm", W=[("ssmp", 2)])
        tr.dma(pool, cnt1[:], cn1[l], "d_cn", W=[("cnt1",)])
        tr.dma(pool, cnt2[:], cn2[l], "d_cn", W=[("cnt2",)])
        tr.dma(sp, brt[:], br[l], "d_br", W=[("brt",)])
        tr.dma(pool, wrt[:], wr[l].rearrange("(k p) n -> p k n", p=128), "d_wr", W=[("wrt",)])
        tr.retire("a1")
        tr.dma(sp, a1[:, 0, :], bstk[l], "d_b", W=[("a1", "w0")])
        tr.dma(sp, a1[:, 1, :], bswp[l], "d_b", W=[("a1", "w1")])
        S = lambda i: ssmp[:, i, :]
        SK = lambda i: ("ssmp", i)
        OP(act, lambda e: e.activation(out=S(3), in_=S(2), func=AF.Exp), R=[SK(2)], W=[SK(3)])
        OP(dve, lambda e: e.tensor_scalar_min(out=S(4), in0=S(0), scalar1=-1e-4), R=[SK(0)], W=[SK(4)])
        OP(dve, lambda e: e.tensor_tensor(out=S(7), in0=S(4), in1=S(3), op=ALU.mult), R=[SK(4), SK(3)], W=[SK(7)])
        OP(act, lambda e: e.activation(out=S(5), in_=S(7), func=AF.Exp), R=[SK(7)], W=[SK(5)])
        OP(dve, lambda e: e.scalar_tensor_tensor(out=S(6), in0=S(1), scalar=1.0 / TWO_PI, in1=S(3),
                                                 op0=ALU.mult, op1=ALU.mult), R=[SK(1), SK(3)], W=[SK(6)])
        OP(dve, lambda e: e.tensor_scalar(out=S(7), in0=S(6), scalar1=MAGIC, scalar2=MAGIC, op0=ALU.add,
                                          op1=ALU.subtract), R=[SK(6)], W=[SK(7)])
        OP(dve, lambda e: e.tensor_tensor(out=S(8), in0=S(6), in1=S(7), op=ALU.subtract), R=[SK(6), SK(7)], W=[SK(8)])
        OP(act, lambda e: e.activation(out=S(9), in_=S(8), func=AF.Abs), R=[SK(8)], W=[SK(9)])
        OP(act, lambda e: e.activation(out=S(10), in_=S(8), func=AF.Sin, scale=TWO_PI), R=[SK(8)], W=[SK(10)])
        OP(act, lambda e: e.activation(out=S(11), in_=S(9), func=AF.Sin, scale=-TWO_PI, bias=cc(C_HPI)),
           R=[SK(9)] + CON, W=[SK(11)])
        OP(dve, lambda e: e.tensor_tensor(out=S(12), in0=S(5), in1=S(11), op=ALU.mult), R=[SK(5), SK(11)], W=[SK(12)])
        OP(dve, lambda e: e.tensor_scalar_add(out=S(12), in0=S(12), scalar1=-1.0), R=[SK(12)], W=[SK(12)])
        OP(dve, lambda e: e.tensor_tensor(out=S(13), in0=S(5), in1=S(10), op=ALU.mult), R=[SK(5), SK(10)], W=[SK(13)])
        OP(dve, lambda e: e.tensor_tensor(out=S(7), in0=S(4), in1=S(4), op=ALU.mult), R=[SK(4)], W=[SK(7)])
        OP(dve, lambda e: e.tensor_tensor(out=S(8), in0=S(1), in1=S(1), op=ALU.mult), R=[SK(1)], W=[SK(8)])
        OP(dve, lambda e: e.tensor_tensor(out=S(7), in0=S(7), in1=S(8), op=ALU.add), R=[SK(7), SK(8)], W=[SK(7)])
        OP(dve, lambda e: e.reciprocal(out=S(7), in_=S(7)), R=[SK(7)], W=[SK(7)])
        OP(dve, lambda e: e.tensor_tensor(out=S(8), in0=S(12), in1=S(4), op=ALU.mult), R=[SK(12), SK(4)], W=[SK(8)])
        OP(dve, lambda e: e.tensor_tensor(out=S(9), in0=S(13), in1=S(1), op=ALU.mult), R=[SK(13), SK(1)], W=[SK(9)])
        OP(dve, lambda e: e.tensor_tensor(out=S(8), in0=S(8), in1=S(9), op=ALU.add), R=[SK(8), SK(9)], W=[SK(8)])
        OP(dve, lambda e: e.tensor_tensor(out=S(14), in0=S(8), in1=S(7), op=ALU.mult), R=[SK(8), SK(7)], W=[SK(14)])
        OP(dve, lambda e: e.tensor_tensor(out=S(8), in0=S(13), in1=S(4), op=ALU.mult), R=[SK(13), SK(4)], W=[SK(8)])
        OP(dve, lambda e: e.tensor_tensor(out=S(9), in0=S(12), in1=S(1), op=ALU.mult), R=[SK(12), SK(1)], W=[SK(9)])
        OP(dve, lambda e: e.tensor_tensor(out=S(8), in0=S(8), in1=S(9), op=ALU.subtract), R=[SK(8), SK(9)], W=[SK(8)])
        OP(dve, lambda e: e.scalar_tensor_tensor(out=S(15), in0=S(8), scalar=cc(C_SGN), in1=S(7),
                                                 op0=ALU.mult, op1=ALU.mult), R=[SK(8), SK(7)] + CON, W=[SK(15)])
        b3 = lambda ap: ap.rearrange("p (g h) -> p g h", h=16)
        crb = ssmp[:, 14, :].unsqueeze(2).to_broadcast([128, 64, 16])
        cib = ssmp[:, 15, :].unsqueeze(2).to_broadcast([128, 64, 16])
        OP(dve, lambda e: e.tensor_tensor(out=b3(a1[:, 0, :]), in0=b3(a1[:, 0, :]), in1=crb, op=ALU.mult),
           R=[("a1", "w0"), SK(14)], W=[("a1", "w0")])
        OP(dve, lambda e: e.tensor_tensor(out=b3(a1[:, 1, :]), in0=b3(a1[:, 1, :]), in1=cib, op=ALU.mult),
           R=[("a1", "w1"), SK(15)], W=[("a1", "w1")])
        OP(dve, lambda e: e.tensor_tensor(out=b1[:], in0=a1[:, 0, :], in1=a1[:, 1, :], op=ALU.subtract),
           R=[("a1", "w0"), ("a1", "w1")], W=[("b1",)])
        OP(dve, lambda e: e.memset(carry[:], 0.0), W=[("carry",)])
        OP(dve, lambda e: e.memset(halo[:], 0.0), W=[("halo",)])

        for h in range(2):
            tr.retire("a1")
            src = xT[h] if l == 0 else xs[h]
            for q4 in range(4):
                tr.dma(sp, xres[:, q4 * 4:(q4 + 1) * 4, :],
                       src[q4 * 512:(q4 + 1) * 512, :].rearrange("(t p) n -> p t n", p=128), "d_x",
                       R=[("xs", h)], W=[("xres", t) for t in range(q4 * 4, q4 * 4 + 4)])
            for t in range(16):
                eng = act if t % 2 == 0 else dve
                if eng == act:
                    OP(act, lambda e, t=t: e.activation(out=xbv[:, t, :], in_=xres[:, t, :], func=AF.Copy),
                       R=[("xres", t)], W=[("a1", "xb", t)])
                else:
                    OP(dve, lambda e, t=t: e.tensor_copy(out=xbv[:, t, :], in_=xres[:, t, :]),
                       R=[("xres", t)], W=[("a1", "xb", t)])
            tr.retire("a2")
            tr.retire("a3")
            for t in range(8):
                OP(act, lambda e, t=t: e.activation(out=a3[:, t, 0:30], in_=halo[:, t, :], func=AF.Copy),
                   R=[("halo",)], W=[("a3", t, "halo")])

            def ev_inproj(mt, tb, bank):
                if mt < 8:
                    OP(act, lambda e: e.activation(out=uv[:, mt, tbs(tb)], in_=ps[:, bank, :], func=AF.Identity,
                                                   bias=vc(V_BIN + mt)),
                       R=[("ps", bank), ("vec",)], W=[("a2", "u", mt, tb)])
                elif mt < 16:
                    t = mt - 8
                    OP(act, lambda e: e.activation(out=a3[:, t, 30 + tb * 512:30 + (tb + 1) * 512], in_=ps[:, bank, :],
                                                   func=AF.Identity, bias=vc(V_BIN + mt)),
                       R=[("ps", bank), ("vec",)], W=[("a3", t, tb)])
                else:
                    t = mt - 16
                    OP(act, lambda e: e.activation(out=tq[:, 0, :], in_=ps[:, bank, :], func=AF.Sigmoid,
                                                   bias=vc(V_BIN + mt)),
                       R=[("ps", bank), ("vec",)], W=[("tq", 0)])
                    OP(dve, lambda e: e.tensor_tensor(out=a3[:, t, 30 + tb * 512:30 + (tb + 1) * 512],
                                                      in0=a3[:, t, 30 + tb * 512:30 + (tb + 1) * 512],
                                                      in1=tq[:, 0, :], op=ALU.mult),
                       R=[("tq", 0), ("a3", t, tb)], W=[("a3", t, tb)])
                    if tb == 1:
                        OP(dve, lambda e: e.tensor_copy(out=halo[:, t, :], in_=a3[:, t, 1024:1054]),
                           R=[("a3", t, 1)], W=[("halo",)])

            gemm(l, h, lambda m0, n: w_in[l].rearrange("(k p) n -> p k n", p=128)[:, :, m0:m0 + n], 16, 24,
                 lambda kt, tb: xbv[:, kt, tbs(tb)], lambda kt, tb: [("a1", "xb", kt)], ev_inproj, 2)

            if h == 1 or l > 0:
                OP(dve, lambda e, d_=(1024.0 if h == 1 else -1024.0): e.tensor_scalar_add(out=con[:, C_IOTA:C_IOTA + 1024],
                                                                                in0=con[:, C_IOTA:C_IOTA + 1024], scalar1=d_),
                   R=CON, W=[("con",)])
            tr.retire("a1")
            WK = lambda i: ("a1", "wk", i)
            wk = lambda i: a1[:, i, :]
            hx = lambda t, tb: a3[:, t, 30 + tb * 512:30 + (tb + 1) * 512]

            def conv_chunk(t, j):
                if j == 0:
                    OP(dve, lambda e: e.tensor_tensor(out=dgt[:], in0=con[:, C_ID:C_ID + 128].unsqueeze(1).to_broadcast([128, 31, 128]),
                                                       in1=wdwt[:, t, :].unsqueeze(2).to_broadcast([128, 31, 128]), op=ALU.mult),
                       R=CON + [("wdw",)], W=[("dg",)])
                for q in range(j * 8, min(62, (j + 1) * 8)):
                    tb, k = (1, q) if q < 31 else (0, q - 31)
                    bank = 0
                    c0 = tb * 512 + k
                    OP(pe, lambda e, k=k, c0=c0, bank=bank: e.matmul(ps[:, bank, :], dgt[:, k, :], a3[:, t, c0:c0 + 512],
                                                                      start=(k == 0), stop=(k == 30)),
                       R=[("dg",), ("a3", t, 0), ("a3", t, 1), ("a3", t, "halo")], W=[("ps", bank)], sig=(k == 30))
                    if k == 30:
                        OP(act, lambda e, tb=tb, bank=bank: e.activation(out=a3[:, t, 30 + tb * 512:30 + (tb + 1) * 512],
                                                                         in_=ps[:, bank, :], func=AF.Identity, bias=vc(V_BDW + t)),
                           R=[("ps", bank), ("vec",)], W=[("a3", t, tb)])

            psb2 = ps[:, 1, 0:64].bitcast(BF16)
            psb3 = ps[:, 1, 64:128].bitcast(BF16)

            def build_tile(Tt):
                bf = Tt % 2
                OP(pe, lambda e: e.transpose(psb2, b1[:, Tt * 128:(Tt + 1) * 128], idb[:]),
                   R=[("b1",), ("idb",)], W=[("ps", 1)])
                for j in range(8):
                    OP(dve, lambda e, j=j: e.tensor_scalar(out=bl[:, bf * 2, j * 128:(j + 1) * 128], in0=psb2,
                                                           scalar1=cc(C_MASK + j), scalar2=None, op0=ALU.mult),
                       R=[("ps", 1)] + CON, W=[("bl", bf, 0, j)])
                    OP(act, lambda e, j=j: e.activation(out=bl[:, bf * 2 + 1, j * 128:j * 128 + 64],
                                                        in_=bl[:, bf * 2, j * 128 + 64:(j + 1) * 128], func=AF.Copy),
                       R=[("bl", bf, 0, j)], W=[("bl", bf, 1, j, 0)])
                    OP(act, lambda e, j=j: e.activation(out=bl[:, bf * 2 + 1, j * 128 + 64:(j + 1) * 128],
                                                        in_=bl[:, bf * 2, j * 128:j * 128 + 64], func=AF.Copy, scale=-1.0),
                       R=[("bl", bf, 0, j)], W=[("bl", bf, 1, j, 1)])
                for v, cnt_ in ((0, cnt1), (1, cnt2)):
                    OP(pe, lambda e, cnt_=cnt_: e.transpose(psb3, cnt_[:, Tt * 128:(Tt + 1) * 128], idb[:]),
                       R=[("cnt1",), ("cnt2",), ("idb",)], W=[("ps", 1)])
                    for j in range(8):
                        if v == 0:
                            OP(dve, lambda e, j=j: e.tensor_scalar(out=cl[:, bf * 2, j * 128 + j * 16:j * 128 + j * 16 + 16],
                                                                   in0=psb3[:, j * 16:j * 16 + 16], scalar1=cc(C_SGN),
                                                                   scalar2=None, op0=ALU.mult),
                               R=[("ps", 1)] + CON, W=[("cl", bf, 0, j)])
                        else:
                            OP(dve, lambda e, j=j: e.tensor_scalar(out=cl[:, bf * 2 + 1, j * 128 + j * 16:j * 128 + j * 16 + 16],
                                                                   in0=psb3[:, j * 16:j * 16 + 16], scalar1=-1.0,
                                                                   scalar2=None, op0=ALU.mult),
                               R=[("ps", 1)], W=[("cl", bf, 1, j)])

            def ev_y(T_, stage):
                y0, y1, t_ = tq[:, 0, :], tq[:, 1, :], tq[:, 2, :]
                ys = (y0, y1)

                def prep(tb):
                    y = ys[tb]
                    OP(dve, lambda e: e.tensor_tensor(out=t_, in0=y, in1=y, op=ALU.mult), R=[("tq", tb)], W=[("tq", 2)])
                    OP(dve, lambda e: e.tensor_scalar(out=t_, in0=t_, scalar1=0.044715, scalar2=1.0, op0=ALU.mult, op1=ALU.add),
                       R=[("tq", 2)], W=[("tq", 2)])
                    OP(dve, lambda e: e.tensor_tensor(out=t_, in0=t_, in1=y, op=ALU.mult), R=[("tq", tb), ("tq", 2)], W=[("tq", 2)])
                    OP(act, lambda e: e.activation(out=t_, in_=t_, func=AF.Sigmoid, scale=1.5957691216057308),
                       R=[("tq", 2)], W=[("tq", 2)])

                def fin(tb):
                    y = ys[tb]
                    OP(dve, lambda e: e.tensor_tensor(out=uv[:, T_, tbs(tb)], in0=y, in1=t_, op=ALU.mult),
                       R=[("tq", tb), ("tq", 2)], W=[("a2", "u", T_, tb)])

                if stage == 0:
                    for tb in range(2):
                        OP(dve, lambda e, tb=tb: e.scalar_tensor_tensor(out=ys[tb], in0=uv[:, T_, tbs(tb)], scalar=vc(V_D + T_),
                                                                        in1=ps[:, 6 + tb, :], op0=ALU.mult, op1=ALU.add),
                           R=[("a2", "u", T_, tb), ("ps", 6 + tb), ("vec",)], W=[("tq", tb)])
                elif stage == 1:
                    prep(0)
                elif stage == 2:
                    fin(0)
                    prep(1)
                else:
                    fin(1)

            def emit_bu(T_, j_):
                bf_ = T_ % 2
                for v in range(2):
                    for tb in range(2):
                        OP(pe, lambda e, v=v, tb=tb: e.matmul(ps[:, 2 + 2 * v + tb, :],
                                                              bl[:, bf_ * 2 + v, j_ * 128:(j_ + 1) * 128],
                                                              uv[:, T_, tbs(tb)], start=True, stop=True),
                           R=[("bl", bf_, v, j_), ("bl", bf_, v, j_, 0), ("bl", bf_, v, j_, 1), ("a2", "u", T_, tb)],
                           W=[("ps", 2 + 2 * v + tb)], sig=(tb == 1))

            build_tile(0)
            emit_bu(0, 0)
            for Tt in range(8):
                bf = Tt % 2
                if Tt + 1 < 8:
                    build_tile(Tt + 1)
                for j in range(8):
                    g = Tt * 8 + j
                    tb_ = g % 2
                    C2, S2 = 3 + 2 * tb_, 4 + 2 * tb_
                    thp_g = ssmp[:, 6, g:g + 1]
                    OP(act, lambda e, thp_g=thp_g: e.activation(out=wk(1), in_=con[:, C_IOTA:C_IOTA + 1024], func=AF.Identity,
                                                                scale=thp_g, bias=cc(C_MAG)),
                       R=CON + [SK(6)], W=[WK(1)])
                    OP(act, lambda e: e.activation(out=wk(1), in_=wk(1), func=AF.Identity, bias=cc(C_NMAG)),
                       R=[WK(1)] + CON, W=[WK(1)])
                    OP(dve, lambda e, thp_g=thp_g: e.scalar_tensor_tensor(out=wk(0), in0=con[:, C_IOTA:C_IOTA + 1024], scalar=thp_g,
                                                                       in1=wk(1), op0=ALU.mult, op1=ALU.subtract),
                       R=CON + [SK(6), WK(1)], W=[WK(0)])
                    if Tt > 0 and j == 0:
                        ev_y(Tt - 1, 0)
                    OP(act, lambda e, S2=S2: e.activation(out=wk(S2), in_=wk(0), func=AF.Sin, scale=TWO_PI),
                       R=[WK(0)], W=[WK(S2)])
                    OP(act, lambda e: e.activation(out=wk(1), in_=wk(0), func=AF.Abs),
                       R=[WK(0)], W=[WK(1)])
                    OP(act, lambda e, C2=C2: e.activation(out=wk(C2), in_=wk(1), func=AF.Sin, scale=-TWO_PI,
                                                          bias=cc(C_HPI)), R=[WK(1)] + CON, W=[WK(C2)])
                    bu1 = ps[:, 2:4, :].rearrange("p a n -> p (a n)")
                    bu2 = ps[:, 4:6, :].rearrange("p a n -> p (a n)")
                    OP(dve, lambda e, C2=C2, bu1=bu1: e.tensor_tensor(out=wk(2), in0=wk(C2), in1=bu1, op=ALU.mult),
                       R=[WK(C2), ("ps", 2), ("ps", 3)], W=[WK(2), ("a1", "x12", 0), ("a1", "x12", 1)])
                    OP(dve, lambda e, S2=S2, bu2=bu2: e.tensor_tensor(out=wk(7), in0=wk(S2), in1=bu2, op=ALU.mult),
                       R=[WK(S2), ("ps", 4), ("ps", 5)], W=[WK(7)])
                    if j < 7:
                        emit_bu(Tt, j + 1)
                    elif Tt < 7:
                        emit_bu(Tt + 1, 0)
                    OP(dve, lambda e: e.tensor_tensor(out=wk(2), in0=wk(2), in1=wk(7), op=ALU.add),
                       R=[WK(2), WK(7)], W=[WK(2)])
                    OP(dve, lambda e, g=g: e.tensor_tensor_scan(out=wk(7), data0=ssmp[:, 5, g:g + 1].to_broadcast([128, 1024]),
                                                                data1=wk(2), initial=carry[:, g:g + 1],
                                                                op0=ALU.mult, op1=ALU.add),
                       R=[WK(2), SK(5), ("carry",)], W=[WK(7)])
                    OP(act, lambda e, g=g: e.activation(out=carry[:, g:g + 1], in_=a1[:, 7, 1023:1024], func=AF.Copy),
                       R=[WK(7)], W=[("carry",)])
                    x12 = a1[:, 2, :].bitcast(BF16).rearrange("p (a n) -> p a n", a=2)
                    OP(dve, lambda e, C2=C2, x12=x12: e.tensor_tensor(out=x12[:, 0, :], in0=wk(C2), in1=wk(7), op=ALU.mult),
                       R=[WK(C2), WK(7)], W=[("a1", "x12", 0), WK(2)])
                    OP(dve, lambda e, S2=S2, x12=x12: e.tensor_tensor(out=x12[:, 1, :], in0=wk(S2), in1=wk(7), op=ALU.mult),
                       R=[WK(S2), WK(7)], W=[("a1", "x12", 1), WK(2)])
                    if Tt > 0 and j < 3:
                        ev_y(Tt - 1, j + 1)
                    conv_chunk(Tt, j)
                    for tb in range(2):
                        for v in range(2):
                            OP(pe, lambda e, v=v, tb=tb, j=j, x12=x12, bf=bf: e.matmul(ps[:, 6 + tb, :], cl[:, bf * 2 + v, j * 128:(j + 1) * 128],
                                                                                 x12[:, v, tbs(tb)],
                                                                                 start=(j == 0 and v == 0), stop=(j == 7 and v == 1)),
                               R=[("cl", bf, v, j), ("a1", "x12", v)], W=[("ps", 6 + tb)], sig=(v == 1))
            for st_ in range(4):
                ev_y(7, st_)

            def ev_glu(mt, tb, bank):
                OP(act, lambda e: e.activation(out=tq[:, 0, :], in_=ps[:, bank, :], func=AF.Sigmoid, bias=vc(V_BGLU + mt)),
                   R=[("ps", bank), ("vec",)], W=[("tq", 0)])
                OP(dve, lambda e: e.tensor_tensor(out=osv[:, mt, tbs(tb)], in0=uv[:, mt, tbs(tb)], in1=tq[:, 0, :], op=ALU.mult),
                   R=[("tq", 0), ("a2", "u", mt, tb)], W=[("a1", "os", mt, tb)])

            tr.retire("a1")
            gemm(l, h, lambda m0, n: w_glu[l].rearrange("(k p) n -> p k n", p=128)[:, :, m0:m0 + n], 8, 8,
                 lambda kt, tb: uv[:, kt, tbs(tb)], lambda kt, tb: [("a2", "u", kt, tb)], ev_glu, 4)

            tr.retire("a2")
            for tb in range(2):
                for t in range(8):
                    sq = a2[:, 2, (t % 2) * 256:(t % 2) * 256 + 256].bitcast(BF16)
                    OP(act, lambda e, t=t, tb=tb, sq=sq: e.activation(out=sq, in_=hx(t, tb), func=AF.Square),
                       R=[("a3", t, tb)], W=[("a2", "sq", t % 2)])
                    OP(pe, lambda e, t=t, tb=tb: e.matmul(ps[:, 4, :], oneb[:], hx(t, tb), start=(t == 0), stop=(t == 7)),
                       R=[("oneb",), ("a3", t, tb)], W=[("ps", 4)], sig=(t == 7))
                    OP(pe, lambda e, t=t, sq=sq: e.matmul(ps[:, 6, :], oneb[:], sq, start=(t == 0), stop=(t == 7)),
                       R=[("oneb",), ("a2", "sq", t % 2)], W=[("ps", 6)], sig=True)
                mean, rstd, nmr = a2[:, 3, 0:512], a2[:, 3, 512:1024], a2[:, 2, 512:1024]
                ln_small(OP, tr, ps, 4, 6, mean, rstd, nmr, 1.0 / 1024.0, ("a2", "m"), ("a2", "r"), ("a2", "n"), cc)
                for t in range(8):
                    OP(dve, lambda e, t=t, tb=tb: e.tensor_tensor(out=tq[:, 1, :], in0=hx(t, tb), in1=rstd, op=ALU.mult),
                       R=[("a3", t, tb), ("a2", "r")], W=[("tq", 1)])
                    OP(dve, lambda e: e.tensor_tensor(out=tq[:, 1, :], in0=tq[:, 1, :], in1=nmr, op=ALU.add),
                       R=[("tq", 1), ("a2", "n")], W=[("tq", 1)])
                    OP(act, lambda e, t=t: e.activation(out=tq[:, 2, :], in_=tq[:, 1, :], func=AF.Identity,
                                                        scale=vc(V_CLG + t), bias=vc(V_CLB + t)),
                       R=[("tq", 1), ("vec",)], W=[("tq", 2)])
                    OP(act, lambda e, t=t, tb=tb: e.activation(out=hx(t, tb), in_=tq[:, 2, :], func=AF.Silu),
                       R=[("tq", 2)], W=[("a3", t, tb)])

            def ev_res(bias_col):
                def ev(mt, tb, bank):
                    OP(act, lambda e: e.activation(out=tq[:, 0, :], in_=ps[:, bank, :], func=AF.Identity, bias=vc(bias_col + mt)),
                       R=[("ps", bank), ("vec",)], W=[("tq", 0)])
                    OP(dve, lambda e: e.scalar_tensor_tensor(out=xres[:, mt, tbs(tb)], in0=xres[:, mt, tbs(tb)], scalar=ALPHA,
                                                             in1=tq[:, 0, :], op0=ALU.mult, op1=ALU.add),
                       R=[("tq", 0), ("xres", mt)], W=[("xres", mt)])
                return ev

            gemm(l, h, lambda m0, n: w_o[l].rearrange("(k p) n -> p k n", p=128)[:, :, m0:m0 + n], 16, 16,
                 lambda kt, tb: (osv[:, kt, tbs(tb)] if kt < 8 else hx(kt - 8, tb)),
                 lambda kt, tb: ([("a1", "os", kt, tb)] if kt < 8 else [("a3", kt - 8, tb)]), ev_res(V_BO), 2)

            def layernorm(gcol, bcol):
                tr.retire("a1")
                tr.retire("a2")
                tr.retire("a3")
                a3f = a3[:, :, :].rearrange("p t n -> p (t n)")
                for t in range(16):
                    sq = a3f[:, (t % 2) * 1024:(t % 2 + 1) * 1024]
                    OP(dve, lambda e, t=t: e.tensor_copy(out=xbv[:, t, :], in_=xres[:, t, :]),
                       R=[("xres", t)], W=[("a1", "xb", t)])
                    OP(act, lambda e, t=t, sq=sq: e.activation(out=sq, in_=xres[:, t, :], func=AF.Square),
                       R=[("xres", t)], W=[("a3", "sq", t % 2)])
                    for tb in range(2):
                        OP(pe, lambda e, t=t, tb=tb: e.matmul(ps[:, 4 + tb, :], oneb[:], xbv[:, t, tbs(tb)], start=(t == 0), stop=(t == 15)),
                           R=[("oneb",), ("a1", "xb", t)], W=[("ps", 4 + tb)], sig=(t == 15))
                        OP(pe, lambda e, t=t, tb=tb, sq=sq: e.matmul(ps[:, 6 + tb, :], oneb[:], sq[:, tbs(tb)], start=(t == 0), stop=(t == 15)),
                           R=[("oneb",), ("a3", "sq", t % 2)], W=[("ps", 6 + tb)], sig=(tb == 1))
                for tb in range(2):
                    ln_small(OP, tr, ps, 4 + tb, 6 + tb, a2[:, 0, tbs(tb)], a2[:, 1, tbs(tb)], a2[:, 2, tbs(tb)], 1.0 / DM,
                             ("a2", "m", tb), ("a2", "r", tb), ("a2", "n", tb), cc)
                for t in range(16):
                    tmp = a3f[:, 4096 + (t % 2) * 2048:4096 + (t % 2) * 2048 + 2048].bitcast(F32)
                    OP(dve, lambda e, t=t, tmp=tmp: e.tensor_tensor(out=tmp, in0=xres[:, t, :], in1=a2[:, 1, :], op=ALU.mult),
                       R=[("xres", t), ("a2", "r", 0), ("a2", "r", 1)], W=[("a3", "tmp", t % 2)])
                    OP(dve, lambda e, tmp=tmp: e.tensor_tensor(out=tmp, in0=tmp, in1=a2[:, 2, :], op=ALU.add),
                       R=[("a3", "tmp", t % 2), ("a2", "n", 0), ("a2", "n", 1)], W=[("a3", "tmp", t % 2)])
                    OP(act, lambda e, t=t, tmp=tmp: e.activation(out=xres[:, t, :], in_=tmp, func=AF.Identity,
                                                                 scale=vc(gcol + t), bias=vc(bcol + t)),
                       R=[("a3", "tmp", t % 2), ("vec",)], W=[("xres", t)])
                    OP(act, lambda e, t=t: e.activation(out=xbv[:, t, :], in_=xres[:, t, :], func=AF.Copy),
                       R=[("xres", t)], W=[("a1", "xb", t)])

            layernorm(V_L1G, V_L1B)

            tr.retire("a2")
            tr.retire("a3")
            XBK = lambda kt: ("a1", "xb", kt)
            for tt in range(8):
                for kt in range(16):
                    OP(pe, lambda e, tt=tt, kt=kt: e.matmul(ps[:, 2, 0:36], xbv[:, kt, tt * 128:(tt + 1) * 128], wrt[:, kt, :],
                                                            start=(kt == 0), stop=(kt == 15)),
                       R=[XBK(kt), ("wrt",)], W=[("ps", 2)], sig=(kt == 15))
                R_ = lambda i, n=36: rt[:, i, 0:n]
                RK = lambda i: ("rt", i)
                OP(dve, lambda e: e.tensor_tensor(out=R_(0), in0=ps[:, 2, 0:36], in1=brt[:], op=ALU.add),
                   R=[("ps", 2), ("brt",)], W=[RK(0)])
                c_ = lambda i: rt[:, 7, i:i + 1]
                OP(dve, lambda e: e.reduce_max(out=c_(0), in_=rt[:, 0, 0:4], axis=AX.X), R=[RK(0)], W=[RK(7)])
                OP(dve, lambda e: e.tensor_scalar_mul(out=c_(1), in0=c_(0), scalar1=-1.0), R=[RK(7)], W=[RK(7)])
                OP(act, lambda e: e.activation(out=rt[:, 1, 0:4], in_=rt[:, 0, 0:4], func=AF.Exp, bias=c_(1)), R=[RK(0), RK(7)], W=[RK(1)])
                OP(dve, lambda e: e.reduce_sum(out=c_(2), in_=rt[:, 1, 0:4], axis=AX.X), R=[RK(1), RK(7)], W=[RK(7)])
                OP(dve, lambda e: e.reciprocal(out=c_(3), in_=c_(2)), R=[RK(7)], W=[RK(7)])
                OP(dve, lambda e: e.tensor_scalar(out=rt[:, 1, 0:4], in0=rt[:, 0, 0:4], scalar1=c_(0), scalar2=None, op0=ALU.is_equal),
                   R=[RK(0), RK(7), RK(1)], W=[RK(1)])
                OP(dve, lambda e: e.tensor_scalar(out=rt[:, 1, 0:4], in0=rt[:, 1, 0:4], scalar1=-1.0, scalar2=1e30, op0=ALU.add, op1=ALU.mult),
                   R=[RK(1)], W=[RK(1)])
                OP(dve, lambda e: e.tensor_tensor(out=rt[:, 2, 0:32].rearrange("p (g e) -> p g e", e=8),
                                                  in0=rt[:, 0, 4:36].rearrange("p (g e) -> p g e", e=8),
                                                  in1=rt[:, 1, 0:4].unsqueeze(2).to_broadcast([128, 4, 8]), op=ALU.add),
                   R=[RK(0), RK(1)], W=[RK(2)])
                OP(dve, lambda e: e.reduce_max(out=c_(4), in_=rt[:, 2, 0:32], axis=AX.X), R=[RK(2), RK(7)], W=[RK(7)])
                OP(dve, lambda e: e.tensor_scalar(out=rt[:, 3, 0:32], in0=rt[:, 2, 0:32], scalar1=c_(4), scalar2=None, op0=ALU.is_equal),
                   R=[RK(2), RK(7)], W=[RK(3)])
                OP(dve, lambda e: e.scalar_tensor_tensor(out=rt[:, 4, 0:32], in0=rt[:, 3, 0:32], scalar=-1e30, in1=rt[:, 2, 0:32],
                                                         op0=ALU.mult, op1=ALU.add), R=[RK(3), RK(2)], W=[RK(4)])
                OP(dve, lambda e: e.reduce_max(out=c_(5), in_=rt[:, 4, 0:32], axis=AX.X), R=[RK(4), RK(7)], W=[RK(7)])
                OP(dve, lambda e: e.tensor_scalar(out=rt[:, 5, 0:32], in0=rt[:, 4, 0:32], scalar1=c_(5), scalar2=None, op0=ALU.is_equal),
                   R=[RK(4), RK(7)], W=[RK(5)])
                OP(dve, lambda e: e.tensor_tensor(out=c_(6), in0=c_(5), in1=c_(4), op=ALU.subtract), R=[RK(7)], W=[RK(7)])
                OP(act, lambda e: e.activation(out=c_(6), in_=c_(6), func=AF.Exp), R=[RK(7)], W=[RK(7)])
                OP(dve, lambda e: e.tensor_scalar_add(out=c_(7), in0=c_(6), scalar1=1.0), R=[RK(7)], W=[RK(7)])
                OP(dve, lambda e: e.reciprocal(out=c_(7), in_=c_(7)), R=[RK(7)], W=[RK(7)])
                OP(dve, lambda e: e.tensor_tensor(out=c_(7), in0=c_(7), in1=c_(3), op=ALU.mult), R=[RK(7)], W=[RK(7)])
                OP(dve, lambda e: e.tensor_tensor(out=c_(8), in0=c_(7), in1=c_(6), op=ALU.mult), R=[RK(7)], W=[RK(7)])
                OP(dve, lambda e: e.tensor_scalar(out=rt[:, 3, 0:32], in0=rt[:, 3, 0:32], scalar1=c_(7), scalar2=None, op0=ALU.mult),
                   R=[RK(3), RK(7)], W=[RK(3)])
                OP(dve, lambda e: e.scalar_tensor_tensor(out=rt[:, 6, 0:32], in0=rt[:, 5, 0:32], scalar=c_(8), in1=rt[:, 3, 0:32],
                                                         op0=ALU.mult, op1=ALU.add), R=[RK(5), RK(3), RK(7)], W=[RK(6)])
                OP(pe, lambda e: e.transpose(ps[0:32, 3, 0:128], rt[:, 6, 0:32], con[:, C_ID:C_ID + 128]),
                   R=[RK(6)] + CON, W=[("ps", 3)])
                OP(act, lambda e, tt=tt: e.activation(out=cwT[:, tt * 128:(tt + 1) * 128], in_=ps[0:32, 3, 0:128], func=AF.Copy),
                   R=[("ps", 3)], W=[("a3", "cwT")])

            for eg in range(8):
                for e8 in range(4):
                    ex = eg * 4 + e8
                    sg_, su_ = wa_slot(), wa_slot()
                    wg_d = wa[:, sg_, :].rearrange("p (k n) -> p k n", k=16)
                    wu_d = wa[:, su_, :].rearrange("p (k n) -> p k n", k=16)
                    tr.dma(pool, wg_d, w_gate[l, ex].rearrange("(k p) n -> p k n", p=128), "d_wa%d" % sg_, W=[("wa", sg_)])
                    tr.dma(pool, wu_d, w_up[l, ex].rearrange("(k p) n -> p k n", p=128), "d_wa%d" % su_, W=[("wa", su_)])
                    OP(dve, lambda e, ex=ex: e.tensor_scalar(out=cwm, in0=cwT, scalar1=con[0:32, C_ID + ex:C_ID + ex + 1],
                                                             scalar2=None, op0=ALU.mult), R=[("a3", "cwT")] + CON, W=[("a3", "cwm")])
                    cnt = 0
                    for ft in range(2):
                        for tb in range(2):
                            bg, bu, bb = cnt % 2, 2 + cnt % 2, 4 + cnt % 2
                            cnt += 1
                            for kt in range(16):
                                OP(pe, lambda e, kt=kt, ft=ft, tb=tb, bg=bg, wg_d=wg_d: e.matmul(ps[:, bg, :], wg_d[:, kt, ft * 128:(ft + 1) * 128],
                                                                                                  xbv[:, kt, tbs(tb)], start=(kt == 0), stop=(kt == 15)),
                                   R=[("wa", sg_), XBK(kt)], W=[("ps", bg)], sig=(kt == 15))
                            for kt in range(16):
                                OP(pe, lambda e, kt=kt, ft=ft, tb=tb, bu=bu, wu_d=wu_d: e.matmul(ps[:, bu, :], wu_d[:, kt, ft * 128:(ft + 1) * 128],
                                                                                                  xbv[:, kt, tbs(tb)], start=(kt == 0), stop=(kt == 15)),
                                   R=[("wa", su_), XBK(kt)], W=[("ps", bu)], sig=(kt == 15))
                            OP(pe, lambda e, tb=tb, bb=bb: e.matmul(ps[:, bb, :], con[0:32, C_ONES:C_ONES + 128],
                                                                    cwm[:, tbs(tb)], start=True, stop=True),
                               R=[("a3", "cwm")] + CON, W=[("ps", bb)])
                            OP(act, lambda e, bg=bg: e.activation(out=tq[:, 0, :], in_=ps[:, bg, :], func=AF.Silu), R=[("ps", bg)], W=[("tq", 0)])
                            OP(dve, lambda e, bu=bu: e.tensor_tensor(out=tq[:, 1, :], in0=tq[:, 0, :], in1=ps[:, bu, :], op=ALU.mult),
                               R=[("tq", 0), ("ps", bu)], W=[("tq", 1)])
                            OP(dve, lambda e, e8=e8, ft=ft, tb=tb, bb=bb: e.tensor_tensor(out=actv(e8, ft)[:, tbs(tb)], in0=tq[:, 1, :], in1=ps[:, bb, :], op=ALU.mult),
                               R=[("tq", 1), ("ps", bb)], W=[("a2", "act", e8, ft, tb)])
                for mt in range(16):
                    s = wb_i[0] % 2
                    wb_i[0] += 1
                    wd = wb[:, s, :].rearrange("p (k n) -> p k n", k=8)
                    tr.dma(pool, wd, w_down[l, eg * 4:(eg + 1) * 4].rearrange("e (k p) n -> p (e k) n", p=128)[:, :, mt * 128:(mt + 1) * 128],
                           "d_wb%d" % s, W=[("wb", s)])
                    for tb in range(2):
                        bank = 6 + tb
                        for e8 in range(4):
                            for kt in range(2):
                                first, last = (e8 == 0 and kt == 0), (e8 == 3 and kt == 1)
                                OP(pe, lambda e, e8=e8, kt=kt, tb=tb, bank=bank, wd=wd, first=first, last=last:
                                   e.matmul(ps[:, bank, :], wd[:, e8 * 2 + kt, :], actv(e8, kt)[:, tbs(tb)], start=first, stop=last),
                                   R=[("wb", s), ("a2", "act", e8, kt, tb)], W=[("ps", bank)], sig=last)
                        if eg == 0:
                            OP(dve, lambda e, mt=mt, tb=tb, bank=bank: e.scalar_tensor_tensor(out=xres[:, mt, tbs(tb)], in0=xres[:, mt, tbs(tb)], scalar=ALPHA,
                                                                                             in1=ps[:, bank, :], op0=ALU.mult, op1=ALU.add),
                               R=[("ps", bank), ("xres", mt)], W=[("xres", mt)])
                        else:
                            OP(dve, lambda e, mt=mt, tb=tb, bank=bank: e.tensor_tensor(out=xres[:, mt, tbs(tb)], in0=xres[:, mt, tbs(tb)],
                                                                                      in1=ps[:, bank, :], op=ALU.add),
                               R=[("ps", bank), ("xres", mt)], W=[("xres", mt)])
            layernorm(V_L2G, V_L2B)

            tr.retire("a2")
            tr.dma(pool, wpd, w_p[l].rearrange("(k p) n -> p k n", p=128), "d_wp", W=[("a2", "wp")])
            tr.dma(pool, ptb, pT[l, h].rearrange("(k p) n -> p k n", p=128), "d_pt", W=[("a2", "ptb")])

            def ev_ple(mt, tb, bank):
                for kt in range(2):
                    OP(pe, lambda e, kt=kt: e.matmul(ps[:, 2 + bank, :], wpd[:, kt, mt * 128:(mt + 1) * 128], ptb[:, kt, tbs(tb)],
                                                     start=(kt == 0), stop=(kt == 1)),
                       R=[("a2", "wp"), ("a2", "ptb")], W=[("ps", 2 + bank)], sig=(kt == 1))
                OP(act, lambda e: e.activation(out=tq[:, 0, :], in_=ps[:, bank, :], func=AF.Sigmoid, bias=vc(V_BPG + mt)),
                   R=[("ps", bank), ("vec",)], W=[("tq", 0)])
                OP(dve, lambda e: e.tensor_tensor(out=tq[:, 1, :], in0=tq[:, 0, :], in1=ps[:, 2 + bank, :], op=ALU.mult),
                   R=[("tq", 0), ("ps", 2 + bank)], W=[("tq", 1)])
                OP(dve, lambda e: e.scalar_tensor_tensor(out=xres[:, mt, tbs(tb)], in0=xres[:, mt, tbs(tb)], scalar=ALPHA,
                                                         in1=tq[:, 1, :], op0=ALU.mult, op1=ALU.add),
                   R=[("tq", 1), ("xres", mt)], W=[("xres", mt)])

            gemm(l, h, lambda m0, n: w_pg[l].rearrange("(k p) n -> p k n", p=128)[:, :, m0:m0 + n], 16, 16,
                 lambda kt, tb: xbv[:, kt, tbs(tb)], lambda kt, tb: [XBK(kt)], ev_ple, 2)
            layernorm(V_L3G, V_L3B)

            dst = outT[h] if l == NL - 1 else xs[h]
            for q4 in range(4):
                tr.dma(sp, dst[q4 * 512:(q4 + 1) * 512, :].rearrange("(t p) n -> p t n", p=128), xres[:, q4 * 4:(q4 + 1) * 4, :],
                       "d_out", R=[("xres", t) for t in range(q4 * 4, q4 * 4 + 4)], W=[("xs", h)] if l < NL - 1 else [("out", h)])

    tr.final_wait(sp)
    for eng in (pe, act, dve, pool):
        tr.final_wait(eng)

    semh = {}
    for s in tr.cnt:
        semh[s] = es.enter_context(nc.semaphore(s))
    with nc.allow_low_precision("bf16 matmul operands, fp32 accumulation"), nc.Block() as block:
        def emit(name, e):
            for waits, fn, tok, inc in tr.q[name]:
                for s, v in waits:
                    e.wait_ge(semh[s], v)
                if fn is not None:
                    ins = fn(e)
                    if tok is not None:
                        ins.then_inc(semh[tok[0]], inc)

        @block.tensor
        def _(e):
            emit("pe", e)

        @block.scalar
        def _(e):
            emit("act", e)

        @block.vector
        def _(e):
            emit("dve", e)

        @block.gpsimd
        def _(e):
            emit("pool", e)

        @block.sync
        def _(e):
            emit("sp", e)
    es.close()
    return nc


def ln_small(OP, tr, ps, b1_, b2_, mean, rstd, nmr, inv_n, km, kr, kn, cc):
    act, dve = "act", "dve"
    OP(act, lambda e: e.activation(out=mean, in_=ps[:, b1_, :], func=AF.Copy, scale=inv_n), R=[("ps", b1_)], W=[km])
    OP(act, lambda e: e.activation(out=rstd, in_=ps[:, b2_, :], func=AF.Copy, scale=inv_n), R=[("ps", b2_)], W=[kr])
    OP(dve, lambda e: e.tensor_tensor(out=nmr, in0=mean, in1=mean, op=ALU.mult), R=[km], W=[kn])
    OP(dve, lambda e: e.tensor_tensor(out=rstd, in0=rstd, in1=nmr, op=ALU.subtract), R=[kr, kn], W=[kr])
    OP(dve, lambda e: e.tensor_scalar(out=rstd, in0=rstd, scalar1=0.0, scalar2=EPS, op0=ALU.max, op1=ALU.add), R=[kr], W=[kr])
    OP(act, lambda e: e.activation(out=rstd, in_=rstd, func=AF.Sqrt), R=[kr], W=[kr])
    OP(dve, lambda e: e.reciprocal(out=rstd, in_=rstd), R=[kr], W=[kr])
    OP(dve, lambda e: e.scalar_tensor_tensor(out=nmr, in0=mean, scalar=-1.0, in1=rstd, op0=ALU.mult, op1=ALU.mult),
       R=[km, kr], W=[kn])


def _host_layout(inp, b):
    f = np.float32
    c = np.ascontiguousarray
    d = {}
    x = inp["x"][b]
    d["xT"] = c(np.stack([x[0:TH].T, x[TH:2 * TH].T]).astype(f))
    p = inp["p"][:, b]
    d["pT"] = c(np.stack([np.stack([p[l, 0:TH].T, p[l, TH:2 * TH].T]) for l in range(NL)]).astype(f))
    for k in ("w_in", "w_glu", "w_o", "w_gate", "w_up", "w_down", "w_p", "w_pg"):
        d[k] = c(inp[k].astype(f))
    d["wr"] = c(np.concatenate([inp["w_rg"], inp["w_re"]], axis=-1).astype(f))
    d["br"] = c(np.broadcast_to(np.concatenate([inp["b_rg"], inp["b_re"]], axis=-1)[:, None, :], (NL, 128, 36)).astype(f))
    cols = lambda v: v.reshape(NL, -1, 128).transpose(0, 2, 1)
    d["vecs"] = c(np.concatenate([cols(inp[k]) for k in ("b_in", "ssm_d", "b_glu", "b_dw", "conv_ln_g", "conv_ln_b", "b_o",
                                                          "ln1_g", "ln1_b", "ln2_g", "ln2_b", "b_pg", "ln3_g", "ln3_b")], axis=-1).astype(f))
    d["wdw"] = c(inp["w_dw"].reshape(NL, 31, 8, 128).transpose(0, 3, 2, 1).reshape(NL, 128, 8 * 31).astype(f))
    st = lambda a: np.concatenate([a, a], axis=1)
    d["lamre"] = c(st(inp["lam_re"].transpose(0, 2, 1)).astype(f))
    d["lamim"] = c(st(inp["lam_im"].transpose(0, 2, 1)).astype(f))
    d["logdt"] = c(np.broadcast_to(inp["log_dt"][:, None, :], (NL, 128, 64)).astype(f))
    bre = inp["ssm_b_re"].transpose(0, 2, 1, 3).reshape(NL, 64, 1024)
    bim = inp["ssm_b_im"].transpose(0, 2, 1, 3).reshape(NL, 64, 1024)
    d["bstk"] = c(np.concatenate([bre, bim], axis=1).astype(f))
    d["bswp"] = c(np.concatenate([bim, bre], axis=1).astype(f))
    cre = inp["ssm_c_re"].reshape(NL, 8, 128, 64)
    cim = inp["ssm_c_im"].reshape(NL, 8, 128, 64)
    d["cn1"] = c(np.concatenate([cre, cim], axis=-1).transpose(0, 2, 1, 3).reshape(NL, 128, 1024).astype(f))
    d["cn2"] = c(np.concatenate([cim, cre], axis=-1).transpose(0, 2, 1, 3).reshape(NL, 128, 1024).astype(f))
    con = np.zeros((128, NCON), f)
    con[:, C_ID:C_ID + 128] = np.eye(128, dtype=f)
    con[:, C_SGN] = np.where(np.arange(128) < 64, 1.0, -1.0)
    for j in range(8):
        con[:, C_MASK + j] = (np.arange(128) // 16 == j)
    con[:, C_IOTA:C_IOTA + 1024] = np.arange(1024, dtype=f)[None, :]
    con[:, C_HPI] = np.pi / 2
    con[:, C_ONE] = 1.0
    con[:, C_ONES:C_ONES + 128] = 1.0
    con[:, C_MAG] = MAGIC
    con[:, C_NMAG] = -MAGIC
    d["consts"] = con
    return d


_NC = None


def kernel(**inputs):
    global _NC
    inp = {k: np.asarray(v) for k, v in inputs.items()}
    if _NC is None:
        _NC = build_program()
    in_maps = [_host_layout(inp, b) for b in range(NCORE)]
    res = run_bass_kernel_spmd(_NC, in_maps, core_ids=list(range(NCORE)))
    out = np.empty((NCORE, 2 * TH, DM), np.float32)
    for b in range(NCORE):
        o = res.results[b]["outT"]
        out[b, 0:TH] = o[0].T
        out[b, TH:] = o[1].T
    return out
```

```python
from contextlib import ExitStack
import math
import numpy as np
import concourse.bass as bass
import concourse.mybir as mybir
from concourse.bass_utils import run_bass_kernel_spmd

F32 = mybir.dt.float32
BF16 = mybir.dt.bfloat16
ALU = mybir.AluOpType
AF = mybir.ActivationFunctionType
AX = mybir.AxisListType

NL = 4
DM = 2048
TH = 1024
NCORE = 4
ALPHA = float((2 * NL) ** 0.25)
EPS = 1e-5
MAGIC = 12582912.0
TWO_PI = float(2 * math.pi)
C_ID, C_SGN, C_MASK, C_IOTA, C_HPI, C_ONE, C_ONES, C_MAG, C_NMAG = 0, 128, 129, 137, 1161, 1162, 1163, 1291, 1292
NCON = 1293
V_BIN, V_D, V_BGLU, V_BDW, V_CLG, V_CLB, V_BO, V_L1G, V_L1B, V_L2G, V_L2B, V_BPG, V_L3G, V_L3B = \
    0, 24, 32, 40, 48, 56, 64, 80, 96, 112, 128, 144, 160, 176
NV = 192
ENGS = ("pe", "act", "dve", "pool", "sp")


class TR:
    def __init__(self):
        self.q = {e: [] for e in ENGS}
        self.cnt = {}
        self.lastw = {}
        self.readers = {}
        self.seen = {e: {} for e in ENGS}
        self.deferred = {e: [] for e in ENGS}
        self.fence = {}
        self.layer = 0

    def engsem(self, eng):
        return "s_%s_%d" % (eng, self.layer)

    def _waits(self, eng, R, W):
        waits = {}

        def need(tok):
            s, v, e = tok
            if e == eng and eng == "pe":
                return
            if self.seen[eng].get(s, 0) >= v:
                return
            if waits.get(s, 0) < v:
                waits[s] = v

        def needd(d):
            for s, (v, e) in d.items():
                need((s, v, e))

        for k in list(R) + list(W):
            f = self.fence.get(k[0])
            if f:
                needd(f)
        for k in R:
            t = self.lastw.get(k)
            if t:
                need(t)
        for k in W:
            t = self.lastw.get(k)
            if t:
                need(t)
            needd(self.readers.get(k, {}))
        for s, v in waits.items():
            self.seen[eng][s] = v
        return list(waits.items())

    def _reg(self, tok, R, W):
        s, v, e = tok
        for k in R:
            self.readers.setdefault(k, {})[s] = (v, e)
        for k in W:
            self.lastw[k] = tok
            self.readers[k] = {}

    def op(self, eng, fn, R=(), W=(), sig=True):
        waits = self._waits(eng, R, W)
        tok = None
        if sig:
            s = self.engsem(eng)
            self.cnt[s] = self.cnt.get(s, 0) + 1
            tok = (s, self.cnt[s], eng)
            for (r_, w_) in self.deferred[eng]:
                self._reg(tok, r_, w_)
            self.deferred[eng] = []
            self._reg(tok, R, W)
        else:
            self.deferred[eng].append((tuple(R), tuple(W)))
        self.q[eng].append((waits, fn, tok, 1))

    def dma(self, eng, out, in_, sem, R=(), W=()):
        waits = self._waits(eng, R, W)
        self.cnt[sem] = self.cnt.get(sem, 0) + 16
        tok = (sem, self.cnt[sem], "dma")
        self._reg(tok, R, W)
        self.q[eng].append((waits, lambda e: e.dma_start(out=out, in_=in_), tok, 16))

    def retire(self, arena):
        f = self.fence.setdefault(arena, {})

        def add(s, v, e):
            if f.get(s, (0, None))[0] < v:
                f[s] = (v, e)

        for k in [k for k in self.lastw if k[0] == arena]:
            s, v, e = self.lastw.pop(k)
            add(s, v, e)
        for k in [k for k in self.readers if k[0] == arena]:
            for s, (v, e) in self.readers.pop(k).items():
                add(s, v, e)

    def final_wait(self, eng):
        waits = []
        for s, v in self.cnt.items():
            if self.seen[eng].get(s, 0) < v:
                waits.append((s, v))
        self.q[eng].append((waits, None, None, 0))


def build_program():
    nc = bass.Bass("TRN2", target_bir_lowering=False)
    tr = TR()
    es = ExitStack()

    def din(name, shape, dt=F32):
        return nc.dram_tensor(name, shape, dt, kind="ExternalInput").ap()

    xT = din("xT", [2, DM, TH])
    pT = din("pT", [NL, 2, 256, TH])
    w_in = din("w_in", [NL, DM, 3072])
    w_glu = din("w_glu", [NL, 1024, 1024])
    w_o = din("w_o", [NL, DM, DM])
    w_gate = din("w_gate", [NL, 32, DM, 256])
    w_up = din("w_up", [NL, 32, DM, 256])
    w_down = din("w_down", [NL, 32, 256, DM])
    w_p = din("w_p", [NL, 256, DM])
    w_pg = din("w_pg", [NL, DM, DM])
    wr = din("wr", [NL, DM, 36])
    br = din("br", [NL, 128, 36])
    vecs = din("vecs", [NL, 128, NV])
    wdw = din("wdw", [NL, 128, 8 * 31])
    lamre = din("lamre", [NL, 128, 64])
    lamim = din("lamim", [NL, 128, 64])
    logdt = din("logdt", [NL, 128, 64])
    bstk = din("bstk", [NL, 128, 1024])
    bswp = din("bswp", [NL, 128, 1024])
    cn1 = din("cn1", [NL, 128, 1024])
    cn2 = din("cn2", [NL, 128, 1024])
    consts = din("consts", [128, NCON])
    outT = nc.dram_tensor("outT", [2, DM, TH], F32, kind="ExternalOutput").ap()
    xs = nc.dram_tensor("xs", [2, DM, TH], F32, kind="Internal").ap()

    def sb(name, shape, dt):
        return es.enter_context(nc.sbuf_tensor(name, shape, dt))

    xres = sb("xres", [128, 16, TH], F32)
    a1 = sb("a1", [128, 8, TH], F32)
    a2 = sb("a2", [128, 4, TH], F32)
    a3 = sb("a3", [128, 8, 1056], BF16)
    wa = sb("wa", [128, 3, 16 * 256], BF16)
    wb = sb("wb", [128, 2, 8 * 128], BF16)
    tq = sb("tq", [128, 3, 512], F32)
    bl = sb("bl", [128, 4, 8 * 128], BF16)
    cl = sb("cl", [128, 4, 8 * 128], BF16)
    con = sb("con", [128, NCON], F32)
    idb = sb("idb", [128, 128], BF16)
    oneb = sb("oneb", [128, 128], BF16)
    vec = sb("vec", [128, NV], F32)
    wdwt = sb("wdwt", [128, 8, 31], F32)
    ssmp = sb("ssmp", [128, 16, 64], F32)
    b1 = sb("b1", [128, 1024], BF16)
    cnt1 = sb("cnt1", [128, 1024], BF16)
    cnt2 = sb("cnt2", [128, 1024], BF16)
    carry = sb("carry", [128, 64], F32)
    halo = sb("halo", [128, 8, 30], BF16)
    wrt = sb("wrt", [128, 16, 36], BF16)
    brt = sb("brt", [128, 36], F32)
    rt = sb("rt", [128, 8, 36], F32)
    dgt = sb("dgt", [128, 31, 128], BF16)
    ps = es.enter_context(nc.psum_tensor("ps", [128, 8, 512], F32))

    a1f = a1
    xbv = a1[:, :, :].rearrange("p t n -> p (t n)").bitcast(BF16).rearrange("p (t n) -> p t n", t=16)
    uv = a2[:, :, :].rearrange("p t n -> p (t n)").bitcast(BF16).rearrange("p (t n) -> p t n", t=8)
    osv = a1[:, 4:8, :].rearrange("p t n -> p (t n)").bitcast(BF16).rearrange("p (t n) -> p t n", t=8)
    a3w = a3[:, :, :].rearrange("p t n -> p (t n)").bitcast(F32)
    cwT, cwm = a3w[0:32, 0:1024], a3w[0:32, 1024:2048]
    ptb = a2[:, 3, :].bitcast(BF16).rearrange("p (k n) -> p k n", k=2)
    wpd = a2[:, 0:2, :].rearrange("p t n -> p (t n)").bitcast(BF16).rearrange("p (k n) -> p k n", k=2)

    def actv(e8, ft):
        return uv[:, e8 * 2 + ft, :]

    def cc(c0, n=1):
        return con[:, c0:c0 + n]

    def vc(c0, n=1):
        return vec[:, c0:c0 + n]

    dve, act, pe, pool, sp = "dve", "act", "pe", "pool", "sp"
    OP = tr.op

    tr.dma(sp, con[:], consts[:], "d_con", W=[("con",)])
    OP(dve, lambda e: e.tensor_copy(out=idb[:], in_=con[:, C_ID:C_ID + 128]), R=[("con",)], W=[("idb",)])
    OP(dve, lambda e: e.memset(oneb[:], 1.0), W=[("oneb",)])
    OP(dve, lambda e: e.memset(cl[:], 0.0), W=[("cl",)])
    OP(dve, lambda e: e.memset(a3[:], 0.0), W=[("a3", "all")])
    CON = [("con",)]

    wa_i = [0]

    def wa_slot():
        i = wa_i[0] % 3
        wa_i[0] += 1
        return i

    wb_i = [0]

    def gemm(l, h, wsrc_fn, nkt, nmt, rhs_fn, rkeys_fn, evac_fn, mt_per_dma, banks=(0, 1)):
        cnt = 0
        for mb in range(0, nmt, mt_per_dma):
            s = wa_slot()
            ncols = mt_per_dma * 128
            dst = wa[:, s, 0:nkt * ncols].rearrange("p (k n) -> p k n", k=nkt)
            tr.dma(pool, dst, wsrc_fn(mb * 128, ncols), "d_wa%d" % s, W=[("wa", s)])
            for mi in range(mt_per_dma):
                mt = mb + mi
                for tb in range(2):
                    bank = banks[cnt % 2]
                    cnt += 1
                    for kt in range(nkt):
                        lhsT = dst[:, kt, mi * 128:(mi + 1) * 128]
                        rhs = rhs_fn(kt, tb)
                        last = kt == nkt - 1
                        OP(pe, (lambda e, o=ps[:, bank, :], a=lhsT, b=rhs, st=(kt == 0), sp_=last:
                                e.matmul(o, a, b, start=st, stop=sp_)),
                           R=[("wa", s)] + rkeys_fn(kt, tb), W=[("ps", bank)], sig=last)
                    evac_fn(mt, tb, bank)

    def tbs(tb):
        return slice(tb * 512, (tb + 1) * 512)

    for l in range(NL):
        tr.layer = l
        tr.dma(sp, vec[:], vecs[l], "d_vec", W=[("vec",)])
        tr.dma(sp, wdwt[:].rearrange("p t k -> p (t k)"), wdw[l], "d_wdw", W=[("wdw",)])
        tr.dma(sp, ssmp[:, 0, :], lamre[l], "d_ssm", W=[("ssmp", 0)])
        tr.dma(sp, ssmp[:, 1, :], lamim[l], "d_ssm", W=[("ssmp", 1)])
        tr.dma(sp, ssmp[:, 2, :], logdt[l], "d_ssm", W=[("ssmp", 2)])
        tr.dma(pool, cnt1[:], cn1[l], "d_cn", W=[("cnt1",)])
        tr.dma(pool, cnt2[:], cn2[l], "d_cn", W=[("cnt2",)])
        tr.dma(sp, brt[:], br[l], "d_br", W=[("brt",)])
        tr.dma(pool, wrt[:], wr[l].rearrange("(k p) n -> p k n", p=128), "d_wr", W=[("wrt",)])
        tr.retire("a1")
        tr.dma(sp, a1[:, 0, :], bstk[l], "d_b", W=[("a1", "w0")])
        tr.dma(sp, a1[:, 1, :], bswp[l], "d_b", W=[("a1", "w1")])
        S = lambda i: ssmp[:, i, :]
        SK = lambda i: ("ssmp", i)
        OP(act, lambda e: e.activation(out=S(3), in_=S(2), func=AF.Exp), R=[SK(2)], W=[SK(3)])
        OP(dve, lambda e: e.tensor_scalar_min(out=S(4), in0=S(0), scalar1=-1e-4), R=[SK(0)], W=[SK(4)])
        OP(dve, lambda e: e.tensor_tensor(out=S(7), in0=S(4), in1=S(3), op=ALU.mult), R=[SK(4), SK(3)], W=[SK(7)])
        OP(act, lambda e: e.activation(out=S(5), in_=S(7), func=AF.Exp), R=[SK(7)], W=[SK(5)])
        OP(dve, lambda e: e.scalar_tensor_tensor(out=S(6), in0=S(1), scalar=1.0 / TWO_PI, in1=S(3),
                                                 op0=ALU.mult, op1=ALU.mult), R=[SK(1), SK(3)], W=[SK(6)])
        OP(dve, lambda e: e.tensor_scalar(out=S(7), in0=S(6), scalar1=MAGIC, scalar2=MAGIC, op0=ALU.add,
                                          op1=ALU.subtract), R=[SK(6)], W=[SK(7)])
        OP(dve, lambda e: e.tensor_tensor(out=S(8), in0=S(6), in1=S(7), op=ALU.subtract), R=[SK(6), SK(7)], W=[SK(8)])
        OP(act, lambda e: e.activation(out=S(9), in_=S(8), func=AF.Abs), R=[SK(8)], W=[SK(9)])
        OP(act, lambda e: e.activation(out=S(10), in_=S(8), func=AF.Sin, scale=TWO_PI), R=[SK(8)], W=[SK(10)])
        OP(act, lambda e: e.activation(out=S(11), in_=S(9), func=AF.Sin, scale=-TWO_PI, bias=cc(C_HPI)),
           R=[SK(9)] + CON, W=[SK(11)])
        OP(dve, lambda e: e.tensor_tensor(out=S(12), in0=S(5), in1=S(11), op=ALU.mult), R=[SK(5), SK(11)], W=[SK(12)])
        OP(dve, lambda e: e.tensor_scalar_add(out=S(12), in0=S(12), scalar1=-1.0), R=[SK(12)], W=[SK(12)])
        OP(dve, lambda e: e.tensor_tensor(out=S(13), in0=S(5), in1=S(10), op=ALU.mult), R=[SK(5), SK(10)], W=[SK(13)])
        OP(dve, lambda e: e.tensor_tensor(out=S(7), in0=S(4), in1=S(4), op=ALU.mult), R=[SK(4)], W=[SK(7)])
        OP(dve, lambda e: e.tensor_tensor(out=S(8), in0=S(1), in1=S(1), op=ALU.mult), R=[SK(1)], W=[SK(8)])
        OP(dve, lambda e: e.tensor_tensor(out=S(7), in0=S(7), in1=S(8), op=ALU.add), R=[SK(7), SK(8)], W=[SK(7)])
        OP(dve, lambda e: e.reciprocal(out=S(7), in_=S(7)), R=[SK(7)], W=[SK(7)])
        OP(dve, lambda e: e.tensor_tensor(out=S(8), in0=S(12), in1=S(4), op=ALU.mult), R=[SK(12), SK(4)], W=[SK(8)])
        OP(dve, lambda e: e.tensor_tensor(out=S(9), in0=S(13), in1=S(1), op=ALU.mult), R=[SK(13), SK(1)], W=[SK(9)])
        OP(dve, lambda e: e.tensor_tensor(out=S(8), in0=S(8), in1=S(9), op=ALU.add), R=[SK(8), SK(9)], W=[SK(8)])
        OP(dve, lambda e: e.tensor_tensor(out=S(14), in0=S(8), in1=S(7), op=ALU.mult), R=[SK(8), SK(7)], W=[SK(14)])
        OP(dve, lambda e: e.tensor_tensor(out=S(8), in0=S(13), in1=S(4), op=ALU.mult), R=[SK(13), SK(4)], W=[SK(8)])
        OP(dve, lambda e: e.tensor_tensor(out=S(9), in0=S(12), in1=S(1), op=ALU.mult), R=[SK(12), SK(1)], W=[SK(9)])
        OP(dve, lambda e: e.tensor_tensor(out=S(8), in0=S(8), in1=S(9), op=ALU.subtract), R=[SK(8), SK(9)], W=[SK(8)])
        OP(dve, lambda e: e.scalar_tensor_tensor(out=S(15), in0=S(8), scalar=cc(C_SGN), in1=S(7),
                                                 op0=ALU.mult, op1=ALU.mult), R=[SK(8), SK(7)] + CON, W=[SK(15)])
        b3 = lambda ap: ap.rearrange("p (g h) -> p g h", h=16)
        crb = ssmp[:, 14, :].unsqueeze(2).to_broadcast([128, 64, 16])
        cib = ssmp[:, 15, :].unsqueeze(2).to_broadcast([128, 64, 16])
        OP(dve, lambda e: e.tensor_tensor(out=b3(a1[:, 0, :]), in0=b3(a1[:, 0, :]), in1=crb, op=ALU.mult),
           R=[("a1", "w0"), SK(14)], W=[("a1", "w0")])
        OP(dve, lambda e: e.tensor_tensor(out=b3(a1[:, 1, :]), in0=b3(a1[:, 1, :]), in1=cib, op=ALU.mult),
           R=[("a1", "w1"), SK(15)], W=[("a1", "w1")])
        OP(dve, lambda e: e.tensor_tensor(out=b1[:], in0=a1[:, 0, :], in1=a1[:, 1, :], op=ALU.subtract),
           R=[("a1", "w0"), ("a1", "w1")], W=[("b1",)])
        OP(dve, lambda e: e.memset(carry[:], 0.0), W=[("carry",)])
        OP(dve, lambda e: e.memset(halo[:], 0.0), W=[("halo",)])

        for h in range(2):
            tr.retire("a1")
            src = xT[h] if l == 0 else xs[h]
            for q4 in range(4):
                tr.dma(sp, xres[:, q4 * 4:(q4 + 1) * 4, :],
                       src[q4 * 512:(q4 + 1) * 512, :].rearrange("(t p) n -> p t n", p=128), "d_x",
                       R=[("xs", h)], W=[("xres", t) for t in range(q4 * 4, q4 * 4 + 4)])
            for t in range(16):
                eng = act if t % 2 == 0 else dve
                if eng == act:
                    OP(act, lambda e, t=t: e.activation(out=xbv[:, t, :], in_=xres[:, t, :], func=AF.Copy),
                       R=[("xres", t)], W=[("a1", "xb", t)])
                else:
                    OP(dve, lambda e, t=t: e.tensor_copy(out=xbv[:, t, :], in_=xres[:, t, :]),
                       R=[("xres", t)], W=[("a1", "xb", t)])
            tr.retire("a2")
            tr.retire("a3")
            for t in range(8):
                OP(act, lambda e, t=t: e.activation(out=a3[:, t, 0:30], in_=halo[:, t, :], func=AF.Copy),
                   R=[("halo",)], W=[("a3", t, "halo")])

            def ev_inproj(mt, tb, bank):
                if mt < 8:
                    OP(act, lambda e: e.activation(out=uv[:, mt, tbs(tb)], in_=ps[:, bank, :], func=AF.Identity,
                                                   bias=vc(V_BIN + mt)),
                       R=[("ps", bank), ("vec",)], W=[("a2", "u", mt, tb)])
                elif mt < 16:
                    t = mt - 8
                    OP(act, lambda e: e.activation(out=a3[:, t, 30 + tb * 512:30 + (tb + 1) * 512], in_=ps[:, bank, :],
                                                   func=AF.Identity, bias=vc(V_BIN + mt)),
                       R=[("ps", bank), ("vec",)], W=[("a3", t, tb)])
                else:
                    t = mt - 16
                    OP(act, lambda e: e.activation(out=tq[:, 0, :], in_=ps[:, bank, :], func=AF.Sigmoid,
                                                   bias=vc(V_BIN + mt)),
                       R=[("ps", bank), ("vec",)], W=[("tq", 0)])
                    OP(dve, lambda e: e.tensor_tensor(out=a3[:, t, 30 + tb * 512:30 + (tb + 1) * 512],
                                                      in0=a3[:, t, 30 + tb * 512:30 + (tb + 1) * 512],
                                                      in1=tq[:, 0, :], op=ALU.mult),
                       R=[("tq", 0), ("a3", t, tb)], W=[("a3", t, tb)])
                    if tb == 1:
                        OP(dve, lambda e: e.tensor_copy(out=halo[:, t, :], in_=a3[:, t, 1024:1054]),
                           R=[("a3", t, 1)], W=[("halo",)])

            gemm(l, h, lambda m0, n: w_in[l].rearrange("(k p) n -> p k n", p=128)[:, :, m0:m0 + n], 16, 24,
                 lambda kt, tb: xbv[:, kt, tbs(tb)], lambda kt, tb: [("a1", "xb", kt)], ev_inproj, 2)

            if h == 1 or l > 0:
                OP(dve, lambda e, d_=(1024.0 if h == 1 else -1024.0): e.tensor_scalar_add(out=con[:, C_IOTA:C_IOTA + 1024],
                                                                                in0=con[:, C_IOTA:C_IOTA + 1024], scalar1=d_),
                   R=CON, W=[("con",)])
            tr.retire("a1")
            WK = lambda i: ("a1", "wk", i)
            wk = lambda i: a1[:, i, :]
            hx = lambda t, tb: a3[:, t, 30 + tb * 512:30 + (tb + 1) * 512]

            def conv_chunk(t, j):
                if j == 0:
                    OP(dve, lambda e: e.tensor_tensor(out=dgt[:], in0=con[:, C_ID:C_ID + 128].unsqueeze(1).to_broadcast([128, 31, 128]),
                                                       in1=wdwt[:, t, :].unsqueeze(2).to_broadcast([128, 31, 128]), op=ALU.mult),
                       R=CON + [("wdw",)], W=[("dg",)])
                for q in range(j * 8, min(62, (j + 1) * 8)):
                    tb, k = (1, q) if q < 31 else (0, q - 31)
                    bank = 0
                    c0 = tb * 512 + k
                    OP(pe, lambda e, k=k, c0=c0, bank=bank: e.matmul(ps[:, bank, :], dgt[:, k, :], a3[:, t, c0:c0 + 512],
                                                                      start=(k == 0), stop=(k == 30)),
                       R=[("dg",), ("a3", t, 0), ("a3", t, 1), ("a3", t, "halo")], W=[("ps", bank)], sig=(k == 30))
                    if k == 30:
                        OP(act, lambda e, tb=tb, bank=bank: e.activation(out=a3[:, t, 30 + tb * 512:30 + (tb + 1) * 512],
                                                                         in_=ps[:, bank, :], func=AF.Identity, bias=vc(V_BDW + t)),
                           R=[("ps", bank), ("vec",)], W=[("a3", t, tb)])

            psb2 = ps[:, 1, 0:64].bitcast(BF16)
            psb3 = ps[:, 1, 64:128].bitcast(BF16)

            def build_tile(Tt):
                bf = Tt % 2
                OP(pe, lambda e: e.transpose(psb2, b1[:, Tt * 128:(Tt + 1) * 128], idb[:]),
                   R=[("b1",), ("idb",)], W=[("ps", 1)])
                for j in range(8):
                    OP(dve, lambda e, j=j: e.tensor_scalar(out=bl[:, bf * 2, j * 128:(j + 1) * 128], in0=psb2,
                                                           scalar1=cc(C_MASK + j), scalar2=None, op0=ALU.mult),
                       R=[("ps", 1)] + CON, W=[("bl", bf, 0, j)])
                    OP(act, lambda e, j=j: e.activation(out=bl[:, bf * 2 + 1, j * 128:j * 128 + 64],
                                                        in_=bl[:, bf * 2, j * 128 + 64:(j + 1) * 128], func=AF.Copy),
                       R=[("bl", bf, 0, j)], W=[("bl", bf, 1, j, 0)])
                    OP(act, lambda e, j=j: e.activation(out=bl[:, bf * 2 + 1, j * 128 + 64:(j + 1) * 128],
                                                        in_=bl[:, bf * 2, j * 128:j * 128 + 64], func=AF.Copy, scale=-1.0),
                       R=[("bl", bf, 0, j)], W=[("bl", bf, 1, j, 1)])
                for v, cnt_ in ((0, cnt1), (1, cnt2)):
                    OP(pe, lambda e, cnt_=cnt_: e.transpose(psb3, cnt_[:, Tt * 128:(Tt + 1) * 128], idb[:]),
                       R=[("cnt1",), ("cnt2",), ("idb",)], W=[("ps", 1)])
                    for j in range(8):
                        if v == 0:
                            OP(dve, lambda e, j=j: e.tensor_scalar(out=cl[:, bf * 2, j * 128 + j * 16:j * 128 + j * 16 + 16],
                                                                   in0=psb3[:, j * 16:j * 16 + 16], scalar1=cc(C_SGN),
                                                                   scalar2=None, op0=ALU.mult),
                               R=[("ps", 1)] + CON, W=[("cl", bf, 0, j)])
                        else:
                            OP(dve, lambda e, j=j: e.tensor_scalar(out=cl[:, bf * 2 + 1, j * 128 + j * 16:j * 128 + j * 16 + 16],
                                                                   in0=psb3[:, j * 16:j * 16 + 16], scalar1=-1.0,
                                                                   scalar2=None, op0=ALU.mult),
                               R=[("ps", 1)], W=[("cl", bf, 1, j)])

            def ev_y(T_, stage):
                y0, y1, t_ = tq[:, 0, :], tq[:, 1, :], tq[:, 2, :]
                ys = (y0, y1)

                def prep(tb):
                    y = ys[tb]
                    OP(dve, lambda e: e.tensor_tensor(out=t_, in0=y, in1=y, op=ALU.mult), R=[("tq", tb)], W=[("tq", 2)])
                    OP(dve, lambda e: e.tensor_scalar(out=t_, in0=t_, scalar1=0.044715, scalar2=1.0, op0=ALU.mult, op1=ALU.add),
                       R=[("tq", 2)], W=[("tq", 2)])
                    OP(dve, lambda e: e.tensor_tensor(out=t_, in0=t_, in1=y, op=ALU.mult), R=[("tq", tb), ("tq", 2)], W=[("tq", 2)])
                    OP(act, lambda e: e.activation(out=t_, in_=t_, func=AF.Sigmoid, scale=1.5957691216057308),
                       R=[("tq", 2)], W=[("tq", 2)])

                def fin(tb):
                    y = ys[tb]
                    OP(dve, lambda e: e.tensor_tensor(out=uv[:, T_, tbs(tb)], in0=y, in1=t_, op=ALU.mult),
                       R=[("tq", tb), ("tq", 2)], W=[("a2", "u", T_, tb)])

                if stage == 0:
                    for tb in range(2):
                        OP(dve, lambda e, tb=tb: e.scalar_tensor_tensor(out=ys[tb], in0=uv[:, T_, tbs(tb)], scalar=vc(V_D + T_),
                                                                        in1=ps[:, 6 + tb, :], op0=ALU.mult, op1=ALU.add),
                           R=[("a2", "u", T_, tb), ("ps", 6 + tb), ("vec",)], W=[("tq", tb)])
                elif stage == 1:
                    prep(0)
                elif stage == 2:
                    fin(0)
                    prep(1)
                else:
                    fin(1)

            def emit_bu(T_, j_):
                bf_ = T_ % 2
                for v in range(2):
                    for tb in range(2):
                        OP(pe, lambda e, v=v, tb=tb: e.matmul(ps[:, 2 + 2 * v + tb, :],
                                                              bl[:, bf_ * 2 + v, j_ * 128:(j_ + 1) * 128],
                                                              uv[:, T_, tbs(tb)], start=True, stop=True),
                           R=[("bl", bf_, v, j_), ("bl", bf_, v, j_, 0), ("bl", bf_, v, j_, 1), ("a2", "u", T_, tb)],
                           W=[("ps", 2 + 2 * v + tb)], sig=(tb == 1))

            def emit_tables(g_):
                tb2 = g_ % 2
                C2_, S2_ = 3 + 2 * tb2, 4 + 2 * tb2
                thp_g = ssmp[:, 6, g_:g_ + 1]
                OP(act, lambda e: e.activation(out=wk(1), in_=con[:, C_IOTA:C_IOTA + 1024], func=AF.Identity,
                                               scale=thp_g, bias=cc(C_MAG)),
                   R=CON + [SK(6)], W=[WK(1)])
                OP(act, lambda e: e.activation(out=wk(1), in_=wk(1), func=AF.Identity, bias=cc(C_NMAG)),
                   R=[WK(1)] + CON, W=[WK(1)])
                OP(dve, lambda e: e.scalar_tensor_tensor(out=wk(0), in0=con[:, C_IOTA:C_IOTA + 1024], scalar=thp_g,
                                                         in1=wk(1), op0=ALU.mult, op1=ALU.subtract),
                   R=CON + [SK(6), WK(1)], W=[WK(0)])
                OP(act, lambda e: e.activation(out=wk(S2_), in_=wk(0), func=AF.Sin, scale=TWO_PI),
                   R=[WK(0)], W=[WK(S2_)])
                OP(act, lambda e: e.activation(out=wk(1), in_=wk(0), func=AF.Abs),
                   R=[WK(0)], W=[WK(1)])
                OP(act, lambda e: e.activation(out=wk(C2_), in_=wk(1), func=AF.Sin, scale=-TWO_PI,
                                               bias=cc(C_HPI)), R=[WK(1)] + CON, W=[WK(C2_)])

            build_tile(0)
            emit_bu(0, 0)
            emit_tables(0)
            for Tt in range(8):
                bf = Tt % 2
                if Tt + 1 < 8:
                    build_tile(Tt + 1)
                for j in range(8):
                    g = Tt * 8 + j
                    tb_ = g % 2
                    C2, S2 = 3 + 2 * tb_, 4 + 2 * tb_
                    if j < 7:
                        emit_tables(Tt * 8 + j + 1)
                    elif Tt < 7:
                        emit_tables((Tt + 1) * 8)
                    if Tt > 0 and j == 0:
                        ev_y(Tt - 1, 0)
                    bu1 = ps[:, 2:4, :].rearrange("p a n -> p (a n)")
                    bu2 = ps[:, 4:6, :].rearrange("p a n -> p (a n)")
                    OP(dve, lambda e, C2=C2, bu1=bu1: e.tensor_tensor(out=wk(2), in0=wk(C2), in1=bu1, op=ALU.mult),
                       R=[WK(C2), ("ps", 2), ("ps", 3)], W=[WK(2), ("a1", "x12", 0), ("a1", "x12", 1)])
                    OP(dve, lambda e, S2=S2, bu2=bu2: e.tensor_tensor(out=wk(7), in0=wk(S2), in1=bu2, op=ALU.mult),
                       R=[WK(S2), ("ps", 4), ("ps", 5)], W=[WK(7)])
                    if j < 7:
                        emit_bu(Tt, j + 1)
                    elif Tt < 7:
                        emit_bu(Tt + 1, 0)
                    OP(dve, lambda e: e.tensor_tensor(out=wk(2), in0=wk(2), in1=wk(7), op=ALU.add),
                       R=[WK(2), WK(7)], W=[WK(2)])
                    OP(dve, lambda e, g=g: e.tensor_tensor_scan(out=wk(7), data0=ssmp[:, 5, g:g + 1].to_broadcast([128, 1024]),
                                                                data1=wk(2), initial=carry[:, g:g + 1],
                                                                op0=ALU.mult, op1=ALU.add),
                       R=[WK(2), SK(5), ("carry",)], W=[WK(7)])
                    OP(act, lambda e, g=g: e.activation(out=carry[:, g:g + 1], in_=a1[:, 7, 1023:1024], func=AF.Copy),
                       R=[WK(7)], W=[("carry",)])
                    x12 = a1[:, 2, :].bitcast(BF16).rearrange("p (a n) -> p a n", a=2)
                    OP(dve, lambda e, C2=C2, x12=x12: e.tensor_tensor(out=x12[:, 0, :], in0=wk(C2), in1=wk(7), op=ALU.mult),
                       R=[WK(C2), WK(7)], W=[("a1", "x12", 0), WK(2)])
                    OP(dve, lambda e, S2=S2, x12=x12: e.tensor_tensor(out=x12[:, 1, :], in0=wk(S2), in1=wk(7), op=ALU.mult),
                       R=[WK(S2), WK(7)], W=[("a1", "x12", 1), WK(2)])
                    if Tt > 0 and j < 3:
                        ev_y(Tt - 1, j + 1)
                    conv_chunk(Tt, j)
                    for tb in range(2):
                        for v in range(2):
                            OP(pe, lambda e, v=v, tb=tb, j=j, x12=x12, bf=bf: e.matmul(ps[:, 6 + tb, :], cl[:, bf * 2 + v, j * 128:(j + 1) * 128],
                                                                                 x12[:, v, tbs(tb)],
                                                                                 start=(j == 0 and v == 0), stop=(j == 7 and v == 1)),
                               R=[("cl", bf, v, j), ("a1", "x12", v)], W=[("ps", 6 + tb)], sig=(v == 1))
            for st_ in range(4):
                ev_y(7, st_)

            def ev_glu(mt, tb, bank):
                OP(act, lambda e: e.activation(out=tq[:, 0, :], in_=ps[:, bank, :], func=AF.Sigmoid, bias=vc(V_BGLU + mt)),
                   R=[("ps", bank), ("vec",)], W=[("tq", 0)])
                OP(dve, lambda e: e.tensor_tensor(out=osv[:, mt, tbs(tb)], in0=uv[:, mt, tbs(tb)], in1=tq[:, 0, :], op=ALU.mult),
                   R=[("tq", 0), ("a2", "u", mt, tb)], W=[("a1", "os", mt, tb)])

            tr.retire("a1")
            gemm(l, h, lambda m0, n: w_glu[l].rearrange("(k p) n -> p k n", p=128)[:, :, m0:m0 + n], 8, 8,
                 lambda kt, tb: uv[:, kt, tbs(tb)], lambda kt, tb: [("a2", "u", kt, tb)], ev_glu, 4)

            tr.retire("a2")
            for tb in range(2):
                for t in range(8):
                    sq = a2[:, 2, (t % 2) * 256:(t % 2) * 256 + 256].bitcast(BF16)
                    OP(act, lambda e, t=t, tb=tb, sq=sq: e.activation(out=sq, in_=hx(t, tb), func=AF.Square),
                       R=[("a3", t, tb)], W=[("a2", "sq", t % 2)])
                    OP(pe, lambda e, t=t, tb=tb: e.matmul(ps[:, 4, :], oneb[:], hx(t, tb), start=(t == 0), stop=(t == 7)),
                       R=[("oneb",), ("a3", t, tb)], W=[("ps", 4)], sig=(t == 7))
                    OP(pe, lambda e, t=t, sq=sq: e.matmul(ps[:, 6, :], oneb[:], sq, start=(t == 0), stop=(t == 7)),
                       R=[("oneb",), ("a2", "sq", t % 2)], W=[("ps", 6)], sig=True)
                mean, rstd, nmr = a2[:, 3, 0:512], a2[:, 3, 512:1024], a2[:, 2, 512:1024]
                ln_small(OP, tr, ps, 4, 6, mean, rstd, nmr, 1.0 / 1024.0, ("a2", "m"), ("a2", "r"), ("a2", "n"), cc)
                for t in range(8):
                    OP(dve, lambda e, t=t, tb=tb: e.tensor_tensor(out=tq[:, 1, :], in0=hx(t, tb), in1=rstd, op=ALU.mult),
                       R=[("a3", t, tb), ("a2", "r")], W=[("tq", 1)])
                    OP(dve, lambda e: e.tensor_tensor(out=tq[:, 1, :], in0=tq[:, 1, :], in1=nmr, op=ALU.add),
                       R=[("tq", 1), ("a2", "n")], W=[("tq", 1)])
                    OP(act, lambda e, t=t: e.activation(out=tq[:, 2, :], in_=tq[:, 1, :], func=AF.Identity,
                                                        scale=vc(V_CLG + t), bias=vc(V_CLB + t)),
                       R=[("tq", 1), ("vec",)], W=[("tq", 2)])
                    OP(act, lambda e, t=t, tb=tb: e.activation(out=hx(t, tb), in_=tq[:, 2, :], func=AF.Silu),
                       R=[("tq", 2)], W=[("a3", t, tb)])

            def ev_res(bias_col):
                def ev(mt, tb, bank):
                    OP(act, lambda e: e.activation(out=tq[:, 0, :], in_=ps[:, bank, :], func=AF.Identity, bias=vc(bias_col + mt)),
                       R=[("ps", bank), ("vec",)], W=[("tq", 0)])
                    OP(dve, lambda e: e.scalar_tensor_tensor(out=xres[:, mt, tbs(tb)], in0=xres[:, mt, tbs(tb)], scalar=ALPHA,
                                                             in1=tq[:, 0, :], op0=ALU.mult, op1=ALU.add),
                       R=[("tq", 0), ("xres", mt)], W=[("xres", mt)])
                return ev

            gemm(l, h, lambda m0, n: w_o[l].rearrange("(k p) n -> p k n", p=128)[:, :, m0:m0 + n], 16, 16,
                 lambda kt, tb: (osv[:, kt, tbs(tb)] if kt < 8 else hx(kt - 8, tb)),
                 lambda kt, tb: ([("a1", "os", kt, tb)] if kt < 8 else [("a3", kt - 8, tb)]), ev_res(V_BO), 2)

            def layernorm(gcol, bcol):
                tr.retire("a1")
                tr.retire("a2")
                tr.retire("a3")
                a3f = a3[:, :, :].rearrange("p t n -> p (t n)")
                for t in range(16):
                    sq = a3f[:, (t % 2) * 1024:(t % 2 + 1) * 1024]
                    OP(dve, lambda e, t=t: e.tensor_copy(out=xbv[:, t, :], in_=xres[:, t, :]),
                       R=[("xres", t)], W=[("a1", "xb", t)])
                    OP(act, lambda e, t=t, sq=sq: e.activation(out=sq, in_=xres[:, t, :], func=AF.Square),
                       R=[("xres", t)], W=[("a3", "sq", t % 2)])
                    for tb in range(2):
                        OP(pe, lambda e, t=t, tb=tb: e.matmul(ps[:, 4 + tb, :], oneb[:], xbv[:, t, tbs(tb)], start=(t == 0), stop=(t == 15)),
                           R=[("oneb",), ("a1", "xb", t)], W=[("ps", 4 + tb)], sig=(t == 15))
                        OP(pe, lambda e, t=t, tb=tb, sq=sq: e.matmul(ps[:, 6 + tb, :], oneb[:], sq[:, tbs(tb)], start=(t == 0), stop=(t == 15)),
                           R=[("oneb",), ("a3", "sq", t % 2)], W=[("ps", 6 + tb)], sig=(tb == 1))
                for tb in range(2):
                    ln_small(OP, tr, ps, 4 + tb, 6 + tb, a2[:, 0, tbs(tb)], a2[:, 1, tbs(tb)], a2[:, 2, tbs(tb)], 1.0 / DM,
                             ("a2", "m", tb), ("a2", "r", tb), ("a2", "n", tb), cc)
                for t in range(16):
                    tmp = a3f[:, 4096 + (t % 2) * 2048:4096 + (t % 2) * 2048 + 2048].bitcast(F32)
                    OP(dve, lambda e, t=t, tmp=tmp: e.tensor_tensor(out=tmp, in0=xres[:, t, :], in1=a2[:, 1, :], op=ALU.mult),
                       R=[("xres", t), ("a2", "r", 0), ("a2", "r", 1)], W=[("a3", "tmp", t % 2)])
                    OP(dve, lambda e, tmp=tmp: e.tensor_tensor(out=tmp, in0=tmp, in1=a2[:, 2, :], op=ALU.add),
                       R=[("a3", "tmp", t % 2), ("a2", "n", 0), ("a2", "n", 1)], W=[("a3", "tmp", t % 2)])
                    OP(act, lambda e, t=t, tmp=tmp: e.activation(out=xres[:, t, :], in_=tmp, func=AF.Identity,
                                                                 scale=vc(gcol + t), bias=vc(bcol + t)),
                       R=[("a3", "tmp", t % 2), ("vec",)], W=[("xres", t)])
                    OP(act, lambda e, t=t: e.activation(out=xbv[:, t, :], in_=xres[:, t, :], func=AF.Copy),
                       R=[("xres", t)], W=[("a1", "xb", t)])

            layernorm(V_L1G, V_L1B)

            tr.retire("a2")
            tr.retire("a3")
            XBK = lambda kt: ("a1", "xb", kt)
            for tt in range(8):
                for kt in range(16):
                    OP(pe, lambda e, tt=tt, kt=kt: e.matmul(ps[:, 2, 0:36], xbv[:, kt, tt * 128:(tt + 1) * 128], wrt[:, kt, :],
                                                            start=(kt == 0), stop=(kt == 15)),
                       R=[XBK(kt), ("wrt",)], W=[("ps", 2)], sig=(kt == 15))
                R_ = lambda i, n=36: rt[:, i, 0:n]
                RK = lambda i: ("rt", i)
                OP(dve, lambda e: e.tensor_tensor(out=R_(0), in0=ps[:, 2, 0:36], in1=brt[:], op=ALU.add),
                   R=[("ps", 2), ("brt",)], W=[RK(0)])
                c_ = lambda i: rt[:, 7, i:i + 1]
                OP(dve, lambda e: e.reduce_max(out=c_(0), in_=rt[:, 0, 0:4], axis=AX.X), R=[RK(0)], W=[RK(7)])
                OP(dve, lambda e: e.tensor_scalar_mul(out=c_(1), in0=c_(0), scalar1=-1.0), R=[RK(7)], W=[RK(7)])
                OP(act, lambda e: e.activation(out=rt[:, 1, 0:4], in_=rt[:, 0, 0:4], func=AF.Exp, bias=c_(1)), R=[RK(0), RK(7)], W=[RK(1)])
                OP(dve, lambda e: e.reduce_sum(out=c_(2), in_=rt[:, 1, 0:4], axis=AX.X), R=[RK(1), RK(7)], W=[RK(7)])
                OP(dve, lambda e: e.reciprocal(out=c_(3), in_=c_(2)), R=[RK(7)], W=[RK(7)])
                OP(dve, lambda e: e.tensor_scalar(out=rt[:, 1, 0:4], in0=rt[:, 0, 0:4], scalar1=c_(0), scalar2=None, op0=ALU.is_equal),
                   R=[RK(0), RK(7), RK(1)], W=[RK(1)])
                OP(dve, lambda e: e.tensor_scalar(out=rt[:, 1, 0:4], in0=rt[:, 1, 0:4], scalar1=-1.0, scalar2=1e30, op0=ALU.add, op1=ALU.mult),
                   R=[RK(1)], W=[RK(1)])
                OP(dve, lambda e: e.tensor_tensor(out=rt[:, 2, 0:32].rearrange("p (g e) -> p g e", e=8),
                                                  in0=rt[:, 0, 4:36].rearrange("p (g e) -> p g e", e=8),
                                                  in1=rt[:, 1, 0:4].unsqueeze(2).to_broadcast([128, 4, 8]), op=ALU.add),
                   R=[RK(0), RK(1)], W=[RK(2)])
                OP(dve, lambda e: e.reduce_max(out=c_(4), in_=rt[:, 2, 0:32], axis=AX.X), R=[RK(2), RK(7)], W=[RK(7)])
                OP(dve, lambda e: e.tensor_scalar(out=rt[:, 3, 0:32], in0=rt[:, 2, 0:32], scalar1=c_(4), scalar2=None, op0=ALU.is_equal),
                   R=[RK(2), RK(7)], W=[RK(3)])
                OP(dve, lambda e: e.scalar_tensor_tensor(out=rt[:, 4, 0:32], in0=rt[:, 3, 0:32], scalar=-1e30, in1=rt[:, 2, 0:32],
                                                         op0=ALU.mult, op1=ALU.add), R=[RK(3), RK(2)], W=[RK(4)])
                OP(dve, lambda e: e.reduce_max(out=c_(5), in_=rt[:, 4, 0:32], axis=AX.X), R=[RK(4), RK(7)], W=[RK(7)])
                OP(dve, lambda e: e.tensor_scalar(out=rt[:, 5, 0:32], in0=rt[:, 4, 0:32], scalar1=c_(5), scalar2=None, op0=ALU.is_equal),
                   R=[RK(4), RK(7)], W=[RK(5)])
                OP(dve, lambda e: e.tensor_tensor(out=c_(6), in0=c_(5), in1=c_(4), op=ALU.subtract), R=[RK(7)], W=[RK(7)])
                OP(act, lambda e: e.activation(out=c_(6), in_=c_(6), func=AF.Exp), R=[RK(7)], W=[RK(7)])
                OP(dve, lambda e: e.tensor_scalar_add(out=c_(7), in0=c_(6), scalar1=1.0), R=[RK(7)], W=[RK(7)])
                OP(dve, lambda e: e.reciprocal(out=c_(7), in_=c_(7)), R=[RK(7)], W=[RK(7)])
                OP(dve, lambda e: e.tensor_tensor(out=c_(7), in0=c_(7), in1=c_(3), op=ALU.mult), R=[RK(7)], W=[RK(7)])
                OP(dve, lambda e: e.tensor_tensor(out=c_(8), in0=c_(7), in1=c_(6), op=ALU.mult), R=[RK(7)], W=[RK(7)])
                OP(dve, lambda e: e.tensor_scalar(out=rt[:, 3, 0:32], in0=rt[:, 3, 0:32], scalar1=c_(7), scalar2=None, op0=ALU.mult),
                   R=[RK(3), RK(7)], W=[RK(3)])
                OP(dve, lambda e: e.scalar_tensor_tensor(out=rt[:, 6, 0:32], in0=rt[:, 5, 0:32], scalar=c_(8), in1=rt[:, 3, 0:32],
                                                         op0=ALU.mult, op1=ALU.add), R=[RK(5), RK(3), RK(7)], W=[RK(6)])
                OP(pe, lambda e: e.transpose(ps[0:32, 3, 0:128], rt[:, 6, 0:32], con[:, C_ID:C_ID + 128]),
                   R=[RK(6)] + CON, W=[("ps", 3)])
                OP(act, lambda e, tt=tt: e.activation(out=cwT[:, tt * 128:(tt + 1) * 128], in_=ps[0:32, 3, 0:128], func=AF.Copy),
                   R=[("ps", 3)], W=[("a3", "cwT")])

            for eg in range(8):
                for e8 in range(4):
                    ex = eg * 4 + e8
                    sg_, su_ = wa_slot(), wa_slot()
                    wg_d = wa[:, sg_, :].rearrange("p (k n) -> p k n", k=16)
                    wu_d = wa[:, su_, :].rearrange("p (k n) -> p k n", k=16)
                    tr.dma(pool, wg_d, w_gate[l, ex].rearrange("(k p) n -> p k n", p=128), "d_wa%d" % sg_, W=[("wa", sg_)])
                    tr.dma(pool, wu_d, w_up[l, ex].rearrange("(k p) n -> p k n", p=128), "d_wa%d" % su_, W=[("wa", su_)])
                    OP(dve, lambda e, ex=ex: e.tensor_scalar(out=cwm, in0=cwT, scalar1=con[0:32, C_ID + ex:C_ID + ex + 1],
                                                             scalar2=None, op0=ALU.mult), R=[("a3", "cwT")] + CON, W=[("a3", "cwm")])
                    cnt = 0
                    for ft in range(2):
                        for tb in range(2):
                            bg, bu, bb = cnt % 2, 2 + cnt % 2, 4 + cnt % 2
                            cnt += 1
                            for kt in range(16):
                                OP(pe, lambda e, kt=kt, ft=ft, tb=tb, bg=bg, wg_d=wg_d: e.matmul(ps[:, bg, :], wg_d[:, kt, ft * 128:(ft + 1) * 128],
                                                                                                  xbv[:, kt, tbs(tb)], start=(kt == 0), stop=(kt == 15)),
                                   R=[("wa", sg_), XBK(kt)], W=[("ps", bg)], sig=(kt == 15))
                            for kt in range(16):
                                OP(pe, lambda e, kt=kt, ft=ft, tb=tb, bu=bu, wu_d=wu_d: e.matmul(ps[:, bu, :], wu_d[:, kt, ft * 128:(ft + 1) * 128],
                                                                                                  xbv[:, kt, tbs(tb)], start=(kt == 0), stop=(kt == 15)),
                                   R=[("wa", su_), XBK(kt)], W=[("ps", bu)], sig=(kt == 15))
                            OP(pe, lambda e, tb=tb, bb=bb: e.matmul(ps[:, bb, :], con[0:32, C_ONES:C_ONES + 128],
                                                                    cwm[:, tbs(tb)], start=True, stop=True),
                               R=[("a3", "cwm")] + CON, W=[("ps", bb)])
                            OP(act, lambda e, bg=bg: e.activation(out=tq[:, 0, :], in_=ps[:, bg, :], func=AF.Silu), R=[("ps", bg)], W=[("tq", 0)])
                            OP(dve, lambda e, bu=bu: e.tensor_tensor(out=tq[:, 1, :], in0=tq[:, 0, :], in1=ps[:, bu, :], op=ALU.mult),
                               R=[("tq", 0), ("ps", bu)], W=[("tq", 1)])
                            OP(dve, lambda e, e8=e8, ft=ft, tb=tb, bb=bb: e.tensor_tensor(out=actv(e8, ft)[:, tbs(tb)], in0=tq[:, 1, :], in1=ps[:, bb, :], op=ALU.mult),
                               R=[("tq", 1), ("ps", bb)], W=[("a2", "act", e8, ft, tb)])
                for mt in range(16):
                    s = wb_i[0] % 2
                    wb_i[0] += 1
                    wd = wb[:, s, :].rearrange("p (k n) -> p k n", k=8)
                    tr.dma(pool, wd, w_down[l, eg * 4:(eg + 1) * 4].rearrange("e (k p) n -> p (e k) n", p=128)[:, :, mt * 128:(mt + 1) * 128],
                           "d_wb%d" % s, W=[("wb", s)])
                    for tb in range(2):
                        bank = 6 + tb
                        for e8 in range(4):
                            for kt in range(2):
                                first, last = (e8 == 0 and kt == 0), (e8 == 3 and kt == 1)
                                OP(pe, lambda e, e8=e8, kt=kt, tb=tb, bank=bank, wd=wd, first=first, last=last:
                                   e.matmul(ps[:, bank, :], wd[:, e8 * 2 + kt, :], actv(e8, kt)[:, tbs(tb)], start=first, stop=last),
                                   R=[("wb", s), ("a2", "act", e8, kt, tb)], W=[("ps", bank)], sig=last)
                        if eg == 0:
                            OP(dve, lambda e, mt=mt, tb=tb, bank=bank: e.scalar_tensor_tensor(out=xres[:, mt, tbs(tb)], in0=xres[:, mt, tbs(tb)], scalar=ALPHA,
                                                                                             in1=ps[:, bank, :], op0=ALU.mult, op1=ALU.add),
                               R=[("ps", bank), ("xres", mt)], W=[("xres", mt)])
                        else:
                            OP(dve, lambda e, mt=mt, tb=tb, bank=bank: e.tensor_tensor(out=xres[:, mt, tbs(tb)], in0=xres[:, mt, tbs(tb)],
                                                                                      in1=ps[:, bank, :], op=ALU.add),
                               R=[("ps", bank), ("xres", mt)], W=[("xres", mt)])
            layernorm(V_L2G, V_L2B)

            tr.retire("a2")
            tr.dma(pool, wpd, w_p[l].rearrange("(k p) n -> p k n", p=128), "d_wp", W=[("a2", "wp")])
            tr.dma(pool, ptb, pT[l, h].rearrange("(k p) n -> p k n", p=128), "d_pt", W=[("a2", "ptb")])

            def ev_ple(mt, tb, bank):
                for kt in range(2):
                    OP(pe, lambda e, kt=kt: e.matmul(ps[:, 2 + bank, :], wpd[:, kt, mt * 128:(mt + 1) * 128], ptb[:, kt, tbs(tb)],
                                                     start=(kt == 0), stop=(kt == 1)),
                       R=[("a2", "wp"), ("a2", "ptb")], W=[("ps", 2 + bank)], sig=(kt == 1))
                OP(act, lambda e: e.activation(out=tq[:, 0, :], in_=ps[:, bank, :], func=AF.Sigmoid, bias=vc(V_BPG + mt)),
                   R=[("ps", bank), ("vec",)], W=[("tq", 0)])
                OP(dve, lambda e: e.tensor_tensor(out=tq[:, 1, :], in0=tq[:, 0, :], in1=ps[:, 2 + bank, :], op=ALU.mult),
                   R=[("tq", 0), ("ps", 2 + bank)], W=[("tq", 1)])
                OP(dve, lambda e: e.scalar_tensor_tensor(out=xres[:, mt, tbs(tb)], in0=xres[:, mt, tbs(tb)], scalar=ALPHA,
                                                         in1=tq[:, 1, :], op0=ALU.mult, op1=ALU.add),
                   R=[("tq", 1), ("xres", mt)], W=[("xres", mt)])

            gemm(l, h, lambda m0, n: w_pg[l].rearrange("(k p) n -> p k n", p=128)[:, :, m0:m0 + n], 16, 16,
                 lambda kt, tb: xbv[:, kt, tbs(tb)], lambda kt, tb: [XBK(kt)], ev_ple, 2)
            layernorm(V_L3G, V_L3B)

            dst = outT[h] if l == NL - 1 else xs[h]
            for q4 in range(4):
                tr.dma(sp, dst[q4 * 512:(q4 + 1) * 512, :].rearrange("(t p) n -> p t n", p=128), xres[:, q4 * 4:(q4 + 1) * 4, :],
                       "d_out", R=[("xres", t) for t in range(q4 * 4, q4 * 4 + 4)], W=[("xs", h)] if l < NL - 1 else [("out", h)])

    tr.final_wait(sp)
    for eng in (pe, act, dve, pool):
        tr.final_wait(eng)

    semh = {}
    for s in tr.cnt:
        semh[s] = es.enter_context(nc.semaphore(s))
    with nc.allow_low_precision("bf16 matmul operands, fp32 accumulation"), nc.Block() as block:
        def emit(name, e):
            for waits, fn, tok, inc in tr.q[name]:
                for s, v in waits:
                    e.wait_ge(semh[s], v)
                if fn is not None:
                    ins = fn(e)
                    if tok is not None:
                        ins.then_inc(semh[tok[0]], inc)

        @block.tensor
        def _(e):
            emit("pe", e)

        @block.scalar
        def _(e):
            emit("act", e)

        @block.vector
        def _(e):
            emit("dve", e)

        @block.gpsimd
        def _(e):
            emit("pool", e)

        @block.sync
        def _(e):
            emit("sp", e)
    es.close()
    return nc


def ln_small(OP, tr, ps, b1_, b2_, mean, rstd, nmr, inv_n, km, kr, kn, cc):
    act, dve = "act", "dve"
    OP(act, lambda e: e.activation(out=mean, in_=ps[:, b1_, :], func=AF.Copy, scale=inv_n), R=[("ps", b1_)], W=[km])
    OP(act, lambda e: e.activation(out=rstd, in_=ps[:, b2_, :], func=AF.Copy, scale=inv_n), R=[("ps", b2_)], W=[kr])
    OP(dve, lambda e: e.tensor_tensor(out=nmr, in0=mean, in1=mean, op=ALU.mult), R=[km], W=[kn])
    OP(dve, lambda e: e.tensor_tensor(out=rstd, in0=rstd, in1=nmr, op=ALU.subtract), R=[kr, kn], W=[kr])
    OP(dve, lambda e: e.tensor_scalar(out=rstd, in0=rstd, scalar1=0.0, scalar2=EPS, op0=ALU.max, op1=ALU.add), R=[kr], W=[kr])
    OP(act, lambda e: e.activation(out=rstd, in_=rstd, func=AF.Sqrt), R=[kr], W=[kr])
    OP(dve, lambda e: e.reciprocal(out=rstd, in_=rstd), R=[kr], W=[kr])
    OP(dve, lambda e: e.scalar_tensor_tensor(out=nmr, in0=mean, scalar=-1.0, in1=rstd, op0=ALU.mult, op1=ALU.mult),
       R=[km, kr], W=[kn])


def _host_layout(inp, b):
    f = np.float32
    c = np.ascontiguousarray
    d = {}
    x = inp["x"][b]
    d["xT"] = c(np.stack([x[0:TH].T, x[TH:2 * TH].T]).astype(f))
    p = inp["p"][:, b]
    d["pT"] = c(np.stack([np.stack([p[l, 0:TH].T, p[l, TH:2 * TH].T]) for l in range(NL)]).astype(f))
    for k in ("w_in", "w_glu", "w_o", "w_gate", "w_up", "w_down", "w_p", "w_pg"):
        d[k] = c(inp[k].astype(f))
    d["wr"] = c(np.concatenate([inp["w_rg"], inp["w_re"]], axis=-1).astype(f))
    d["br"] = c(np.broadcast_to(np.concatenate([inp["b_rg"], inp["b_re"]], axis=-1)[:, None, :], (NL, 128, 36)).astype(f))
    cols = lambda v: v.reshape(NL, -1, 128).transpose(0, 2, 1)
    d["vecs"] = c(np.concatenate([cols(inp[k]) for k in ("b_in", "ssm_d", "b_glu", "b_dw", "conv_ln_g", "conv_ln_b", "b_o",
                                                          "ln1_g", "ln1_b", "ln2_g", "ln2_b", "b_pg", "ln3_g", "ln3_b")], axis=-1).astype(f))
    d["wdw"] = c(inp["w_dw"].reshape(NL, 31, 8, 128).transpose(0, 3, 2, 1).reshape(NL, 128, 8 * 31).astype(f))
    st = lambda a: np.concatenate([a, a], axis=1)
    d["lamre"] = c(st(inp["lam_re"].transpose(0, 2, 1)).astype(f))
    d["lamim"] = c(st(inp["lam_im"].transpose(0, 2, 1)).astype(f))
    d["logdt"] = c(np.broadcast_to(inp["log_dt"][:, None, :], (NL, 128, 64)).astype(f))
    bre = inp["ssm_b_re"].transpose(0, 2, 1, 3).reshape(NL, 64, 1024)
    bim = inp["ssm_b_im"].transpose(0, 2, 1, 3).reshape(NL, 64, 1024)
    d["bstk"] = c(np.concatenate([bre, bim], axis=1).astype(f))
    d["bswp"] = c(np.concatenate([bim, bre], axis=1).astype(f))
    cre = inp["ssm_c_re"].reshape(NL, 8, 128, 64)
    cim = inp["ssm_c_im"].reshape(NL, 8, 128, 64)
    d["cn1"] = c(np.concatenate([cre, cim], axis=-1).transpose(0, 2, 1, 3).reshape(NL, 128, 1024).astype(f))
    d["cn2"] = c(np.concatenate([cim, cre], axis=-1).transpose(0, 2, 1, 3).reshape(NL, 128, 1024).astype(f))
    con = np.zeros((128, NCON), f)
    con[:, C_ID:C_ID + 128] = np.eye(128, dtype=f)
    con[:, C_SGN] = np.where(np.arange(128) < 64, 1.0, -1.0)
    for j in range(8):
        con[:, C_MASK + j] = (np.arange(128) // 16 == j)
    con[:, C_IOTA:C_IOTA + 1024] = np.arange(1024, dtype=f)[None, :]
    con[:, C_HPI] = np.pi / 2
    con[:, C_ONE] = 1.0
    con[:, C_ONES:C_ONES + 128] = 1.0
    con[:, C_MAG] = MAGIC
    con[:, C_NMAG] = -MAGIC
    d["consts"] = con
    return d


_NC = None


def kernel(**inputs):
    global _NC
    inp = {k: np.asarray(v) for k, v in inputs.items()}
    if _NC is None:
        _NC = build_program()
    in_maps = [_host_layout(inp, b) for b in range(NCORE)]
    res = run_bass_kernel_spmd(_NC, in_maps, core_ids=list(range(NCORE)))
    out = np.empty((NCORE, 2 * TH, DM), np.float32)
    for b in range(NCORE):
        o = res.results[b]["outT"]
        out[b, 0:TH] = o[0].T
        out[b, TH:] = o[1].T
    return out
```
